# Optimizing a Trainium2 kernel written in Bass

```python
import jax, jax.numpy as jnp
from jax import lax
import numpy as np

D_MODEL = 4096
BATCH = 2
SEQ = 8192
DEPTH = 2

N_A_LAYERS = DEPTH // 2
N_B_LAYERS = DEPTH - N_A_LAYERS
CONV_WIDTH = 3
HEAD_DIM = 64
N_HEADS = D_MODEL // HEAD_DIM
N_KV_HEADS = 8
GROUP = N_HEADS // N_KV_HEADS
WINDOW = 128
BLOCK = 128
ATTN_SCALE = HEAD_DIM ** -0.5
N_EXPERTS = 32
TOP_K = 4
D_EXPERT = 512
SWIGLU_LIMIT = 7.0
SWIGLU_ALPHA = 1.702
EXPERT_ROWS = 128
EPS = 1e-5

kernel_name = "hybrid_shortconv_sinkswa_yoco_moe"


def rms_norm(x, g):
    xf = x.astype(jnp.float32)
    y = xf * lax.rsqrt(jnp.mean(xf * xf, axis=-1, keepdims=True) + EPS) * g.astype(jnp.float32)
    return y.astype(x.dtype)


def modulate(h, shift, scale):
    return h * (1.0 + scale[:, None, :]) + shift[:, None, :]


def short_conv_mixer(h, w_in, w_conv, w_out):
    S = h.shape[1]
    b_gate, c_gate, u = jnp.split(h @ w_in, 3, axis=-1)
    v = c_gate * u
    vp = jnp.pad(v, ((0, 0), (CONV_WIDTH - 1, 0), (0, 0)))
    conv = sum(w_conv[j] * vp[:, j:j + S] for j in range(CONV_WIDTH))
    return (b_gate * conv) @ w_out


def shared_kv(x, cs, g_kv, w_mod_kv, b_mod_kv, w_kv, b_kv, g_k):
    B, S, _ = x.shape
    shift, scale = jnp.split(cs @ w_mod_kv + b_mod_kv, 2, axis=-1)
    h = modulate(rms_norm(x, g_kv), shift, scale)
    k, v = jnp.split(h @ w_kv + b_kv, 2, axis=-1)
    k = rms_norm(k.reshape(B, S, N_KV_HEADS, HEAD_DIM), g_k)
    v = v.reshape(B, S, N_KV_HEADS, HEAD_DIM)
    return k, v


def band(t):
    B, S = t.shape[:2]
    tb = t.reshape(B, S // BLOCK, BLOCK, N_KV_HEADS, HEAD_DIM)
    prev = jnp.pad(tb, ((0, 0), (1, 0), (0, 0), (0, 0), (0, 0)))[:, :-1]
    return jnp.concatenate([prev, tb], axis=2)


def band_mask(nb):
    qi = jnp.arange(BLOCK)[:, None]
    kj = jnp.arange(2 * BLOCK)[None, :]
    dist = qi + BLOCK - kj
    in_window = (dist >= 0) & (dist < WINDOW)
    not_pad = (jnp.arange(nb)[:, None, None] > 0) | (kj[None] >= BLOCK)
    return in_window[None] & not_pad


def sink_window_attention(h, k_band, v_band, mask, w_q, b_q, g_q, sinks, w_o, b_o):
    B, S, D = h.shape
    nb = S // BLOCK
    q = (h @ w_q + b_q).reshape(B, nb, BLOCK, N_KV_HEADS, GROUP, HEAD_DIM)
    q = rms_norm(q, g_q)
    sink_l = sinks.astype(jnp.float32).reshape(1, N_KV_HEADS, GROUP, 1, 1)

    def one_seq(args):
        qs, ks, vs = args
        s = jnp.einsum('nqhgd,nkhd->nhgqk', qs, ks,
                       preferred_element_type=jnp.float32) * ATTN_SCALE
        s = jnp.where(mask[:, None, None], s, -jnp.inf)
        m = jnp.maximum(jnp.max(s, axis=-1, keepdims=True), sink_l)
        p = jnp.exp(s - m)
        denom = jnp.sum(p, axis=-1, keepdims=True) + jnp.exp(sink_l - m)
        return jnp.einsum('nhgqk,nkhd->nqhgd', (p / denom).astype(vs.dtype), vs)

    o = lax.map(one_seq, (q, k_band, v_band))
    return o.reshape(B, S, D) @ w_o + b_o


def clamped_swiglu(gu):
    glu = jnp.minimum(gu[..., ::2], SWIGLU_LIMIT)
    lin = jnp.clip(gu[..., 1::2], -SWIGLU_LIMIT, SWIGLU_LIMIT)
    return glu * jax.nn.sigmoid(SWIGLU_ALPHA * glu) * (lin + 1.0)


def moe_ffn(h, w_r, b_r, w_gu, b_gu, w_dn, b_dn):
    N, D = h.shape
    logits = (h @ w_r + b_r).astype(jnp.float32)
    top_vals, top_idx = lax.top_k(logits, TOP_K)
    gates = jax.nn.softmax(top_vals, axis=-1)
    A = N * TOP_K
    flat_e = top_idx.reshape(A)
    flat_tok = jnp.arange(A, dtype=jnp.int32) // TOP_K
    order = jnp.argsort(flat_e)
    sorted_e = flat_e[order]
    counts = jax.ops.segment_sum(jnp.ones((A,), jnp.int32), flat_e, num_segments=N_EXPERTS)
    padded = (counts + EXPERT_ROWS - 1) // EXPERT_ROWS * EXPERT_ROWS
    pad_end = jnp.cumsum(padded)
    pad_start = pad_end - padded
    start = jnp.cumsum(counts) - counts
    dest = pad_start[sorted_e] + jnp.arange(A, dtype=jnp.int32) - start[sorted_e]
    n_rows = -(-(A + N_EXPERTS * (EXPERT_ROWS - 1)) // EXPERT_ROWS) * EXPERT_ROWS
    n_blocks = n_rows // EXPERT_ROWS
    row_tok = jnp.full((n_rows,), N, jnp.int32).at[dest].set(flat_tok[order])
    row_gate = jnp.zeros((n_rows,), jnp.float32).at[dest].set(gates.reshape(A)[order])
    block_e = jnp.minimum(
        jnp.searchsorted(pad_end, jnp.arange(n_blocks, dtype=jnp.int32) * EXPERT_ROWS, side='right'),
        N_EXPERTS - 1)
    h_pad = jnp.concatenate([h, jnp.zeros((1, D), h.dtype)], axis=0)

    def run_block(args):
        tok, gate, e = args
        xb = h_pad[tok]
        act = clamped_swiglu(xb @ w_gu[e] + b_gu[e])
        return (act @ w_dn[e] + b_dn[e]) * gate[:, None].astype(xb.dtype)

    y_rows = lax.map(run_block, (row_tok.reshape(n_blocks, EXPERT_ROWS),
                                 row_gate.reshape(n_blocks, EXPERT_ROWS), block_e))
    out = jax.ops.segment_sum(y_rows.reshape(n_rows, D), row_tok, num_segments=N + 1)
    return out[:N]


def setup_inputs(seed: int = 0) -> dict:
    key = jax.random.key(seed)
    ks = jax.random.split(key, 32)
    f32 = jnp.float32
    D, F, E = D_MODEL, D_EXPERT, N_EXPERTS
    KV = 2 * N_KV_HEADS * HEAD_DIM

    def nrm(k, shape, scale):
        return jax.random.normal(k, shape, f32) * scale

    return {
        "x": nrm(ks[0], (BATCH, SEQ, D), 1.0),
        "c": nrm(ks[1], (BATCH, D), 1.0),
        "g_mix": 1.0 + nrm(ks[2], (DEPTH, D), 0.02),
        "w_mod_mix": nrm(ks[3], (DEPTH, D, 3 * D), 0.5 * D ** -0.5),
        "b_mod_mix": nrm(ks[4], (DEPTH, 3 * D), 0.01),
        "g_ffn": 1.0 + nrm(ks[5], (DEPTH, D), 0.02),
        "w_mod_ffn": nrm(ks[6], (DEPTH, D, 3 * D), 0.5 * D ** -0.5),
        "b_mod_ffn": nrm(ks[7], (DEPTH, 3 * D), 0.01),
        "w_a_in": nrm(ks[8], (N_A_LAYERS, D, 3 * D), D ** -0.5),
        "w_a_conv": nrm(ks[9], (N_A_LAYERS, CONV_WIDTH, D), CONV_WIDTH ** -0.5),
        "w_a_out": nrm(ks[10], (N_A_LAYERS, D, D), D ** -0.5),
        "g_kv": 1.0 + nrm(ks[11], (D,), 0.02),
        "w_mod_kv": nrm(ks[12], (D, 2 * D), 0.5 * D ** -0.5),
        "b_mod_kv": nrm(ks[13], (2 * D,), 0.01),
        "w_kv": nrm(ks[14], (D, KV), D ** -0.5),
        "b_kv": nrm(ks[15], (KV,), 0.01),
        "g_k": 1.0 + nrm(ks[16], (HEAD_DIM,), 0.02),
        "w_b_q": nrm(ks[17], (N_B_LAYERS, D, N_HEADS * HEAD_DIM), D ** -0.5),
        "b_b_q": nrm(ks[18], (N_B_LAYERS, N_HEADS * HEAD_DIM), 0.01),
        "g_q": 1.0 + nrm(ks[19], (N_B_LAYERS, HEAD_DIM), 0.02),
        "sinks": nrm(ks[20], (N_B_LAYERS, N_HEADS), 0.5),
        "w_b_o": nrm(ks[21], (N_B_LAYERS, N_HEADS * HEAD_DIM, D), (N_HEADS * HEAD_DIM) ** -0.5),
        "b_b_o": nrm(ks[22], (N_B_LAYERS, D), 0.01),
        "w_router": nrm(ks[23], (DEPTH, D, E), D ** -0.5),
        "b_router": nrm(ks[24], (DEPTH, E), 0.01),
        "w_gu": nrm(ks[25], (DEPTH, E, D, 2 * F), D ** -0.5),
        "b_gu": nrm(ks[26], (DEPTH, E, 2 * F), 0.01),
        "w_dn": nrm(ks[27], (DEPTH, E, F, D), F ** -0.5),
        "b_dn": nrm(ks[28], (DEPTH, E, D), 0.01),
    }


def reference(x, c, g_mix, w_mod_mix, b_mod_mix, g_ffn, w_mod_ffn, b_mod_ffn,
              w_a_in, w_a_conv, w_a_out, g_kv, w_mod_kv, b_mod_kv, w_kv, b_kv, g_k,
              w_b_q, b_b_q, g_q, sinks, w_b_o, b_b_o,
              w_router, b_router, w_gu, b_gu, w_dn, b_dn):
    B, S, D = x.shape
    cs = jax.nn.silu(c)
    mask = band_mask(S // BLOCK)
    k_band = v_band = None
    for layer in range(DEPTH):
        if layer == N_A_LAYERS:
            k, v = shared_kv(x, cs, g_kv, w_mod_kv, b_mod_kv, w_kv, b_kv, g_k)
            k_band, v_band = band(k), band(v)
        shift, scale, gate = jnp.split(cs @ w_mod_mix[layer] + b_mod_mix[layer], 3, axis=-1)
        h = modulate(rms_norm(x, g_mix[layer]), shift, scale)
        if layer < N_A_LAYERS:
            y = short_conv_mixer(h, w_a_in[layer], w_a_conv[layer], w_a_out[layer])
        else:
            j = layer - N_A_LAYERS
            y = sink_window_attention(h, k_band, v_band, mask, w_b_q[j], b_b_q[j], g_q[j],
                                      sinks[j], w_b_o[j], b_b_o[j])
        x = x + gate[:, None, :] * y
        shift, scale, gate = jnp.split(cs @ w_mod_ffn[layer] + b_mod_ffn[layer], 3, axis=-1)
        h = modulate(rms_norm(x, g_ffn[layer]), shift, scale)
        y = moe_ffn(h.reshape(B * S, D), w_router[layer], b_router[layer],
                    w_gu[layer], b_gu[layer], w_dn[layer], b_dn[layer]).reshape(B, S, D)
        x = x + gate[:, None, :] * y
    return x
```

```python
from contextlib import ExitStack
import numpy as np
import concourse.bass as bass
import concourse.mybir as mybir
from concourse.bass_utils import run_bass_kernel_spmd

F32 = mybir.dt.float32
BF16 = mybir.dt.bfloat16
AF = mybir.ActivationFunctionType
ALU = mybir.AluOpType

D = 4096
KC = 32
FH = 512
HD = 64
NH = 64
NKV = 8
EPS = 1e-5


class Buf:
    def __init__(self):
        self.w = {}
        self.r = {}

    def rd_deps(self):
        return list(self.w.items())

    def wr_deps(self):
        return list(self.w.items()) + list(self.r.items())

    def did_read(self, t):
        self.r[t[0]] = max(self.r.get(t[0], 0), t[1])

    def did_write(self, t):
        self.w[t[0]] = max(self.w.get(t[0], 0), t[1])


class Sched:
    ENG = ("pe", "act", "dve", "pool", "sp")
    BLK = {"pe": "tensor", "act": "scalar", "dve": "vector", "pool": "gpsimd", "sp": "sync"}

    def __init__(self, nc, tag):
        self.nc = nc
        self.tag = tag
        self.q = {e: [] for e in self.ENG}
        self.cnt = {}
        self.seen = {e: {} for e in self.ENG}
        self.step = {}

    def _waits(self, eng, deps):
        need = {}
        for d in deps:
            if d is None:
                continue
            need[d[0]] = max(need.get(d[0], 0), d[1])
        out = []
        for k, v in need.items():
            if self.seen[eng].get(k, 0) < v:
                self.seen[eng][k] = v
                out.append((k, v))
        return out

    def op(self, eng, fn, deps=(), mark=True, dma=None, step=16):
        waits = self._waits(eng, deps)
        tok = None
        key = None
        if dma is not None:
            key = "D" + dma
            self.step[key] = step
            self.cnt[key] = self.cnt.get(key, 0) + step
            tok = (key, self.cnt[key])
        elif mark:
            key = "E" + eng
            self.step[key] = 1
            self.cnt[key] = self.cnt.get(key, 0) + 1
            tok = (key, self.cnt[key])
        self.q[eng].append((waits, fn, key))
        return tok

    def run(self, eng, fns, reads=(), writes=(), dma=None, step=16):
        if not isinstance(fns, list):
            fns = [fns]
        deps = []
        for b in reads:
            deps += b.rd_deps()
        for b in writes:
            deps += b.wr_deps()
        for f in fns[:-1]:
            self.op(eng, f, deps, mark=False)
            deps = ()
        tok = self.op(eng, fns[-1], deps, dma=dma, step=step)
        for b in reads:
            b.did_read(tok)
        for b in writes:
            b.did_write(tok)
        return tok

    def finish(self):
        allt = [(k, v) for k, v in self.cnt.items()]
        for e in self.ENG:
            waits = self._waits(e, allt)
            if waits:
                self.q[e].append((waits, None, None))

    def emit(self):
        nc = self.nc
        self.finish()
        sems = {}
        for k in self.cnt:
            sems[k] = nc.alloc_semaphore(name=self.tag + k)
        with nc.Block() as block:
            for e in self.ENG:
                if not self.q[e]:
                    continue

                def body(eo, e=e):
                    for waits, fn, key in self.q[e]:
                        for (k, v) in waits:
                            eo.wait_ge(sems[k], v)
                        if fn is None:
                            continue
                        ins = fn(eo)
                        if key is not None:
                            ins.then_inc(sems[key], self.step[key])

                getattr(block, self.BLK[e])(body)
        nc.all_engine_barrier()
        nc.clear_and_free_semaphores(list(sems.values()))
        nc.all_engine_barrier()


class Ring:
    def __init__(self, st, nc, name, n, shape, dtype):
        self.t = [st.enter_context(nc.sbuf_tensor(f"{name}{i}", shape, dtype)) for i in range(n)]
        self.b = [Buf() for _ in range(n)]
        self.n = n
        self.i = 0
        self.name = name

    def next(self):
        i = self.i % self.n
        self.i += 1
        return self.t[i], self.b[i], f"{self.name}{i}"


def psum_banks(st, nc, tag, n=8):
    ts = [st.enter_context(nc.psum_tensor(f"{tag}ps{i}", [128, 512], F32)) for i in range(n)]
    return ts, [Buf() for _ in range(n)]


def stage_gather(nc, ncores, pairs):
    S = Sched(nc, "g")
    for i, (ext, bounce, full) in enumerate(pairs):
        b = Buf()
        S.run("pool", lambda e, ext=ext, bounce=bounce: e.dma_start(out=bounce.ap(), in_=ext), writes=[b], dma="b")
        S.run("pool", lambda e, bounce=bounce, full=full: e.collective_compute(
            "AllGather", ALU.bypass, replica_groups=[list(range(ncores))],
            ins=[bounce.ap().opt()], outs=[full.ap().opt()]), reads=[b], dma="cc", step=1)
    S.emit()


def stage_mod(nc, tag, wm, ncol, cT, gT_ap, bmT_ap, bgate_ap, at_out, bt_out, g2_out):
    has_gate = g2_out is not None
    nblk = ncol // 128
    with ExitStack() as st:
        S = Sched(nc, tag)
        ps, pb = psum_banks(st, nc, tag)
        ring = Ring(st, nc, tag + "wm", 3, [128, KC, 128], F32)
        csT = st.enter_context(nc.sbuf_tensor(tag + "csT", [128, KC], F32))
        c_in = st.enter_context(nc.sbuf_tensor(tag + "cin", [128, KC], F32))
        gT = st.enter_context(nc.sbuf_tensor(tag + "gT", [128, KC], F32))
        bmT = st.enter_context(nc.sbuf_tensor(tag + "bmT", [128, 64], F32))
        modc = st.enter_context(nc.sbuf_tensor(tag + "modc", [128, 64], F32))
        at = st.enter_context(nc.sbuf_tensor(tag + "at", [128, KC], F32))
        bt = st.enter_context(nc.sbuf_tensor(tag + "bt", [128, KC], F32))
        b_small = Buf()
        S.run("sp", lambda e: e.dma_start(out=c_in[:], in_=cT), writes=[b_small], dma="s")
        S.run("sp", lambda e: e.dma_start(out=gT[:], in_=gT_ap), writes=[b_small], dma="s")
        S.run("sp", lambda e: e.dma_start(out=bmT[:], in_=bmT_ap), writes=[b_small], dma="s")
        b_cs = Buf()
        S.run("act", lambda e: e.activation(out=csT[:], in_=c_in[:], func=AF.Silu), reads=[b_small], writes=[b_cs])
        if has_gate:
            csrep = st.enter_context(nc.sbuf_tensor(tag + "csrep", [128, KC, 128], F32))
            zeros = st.enter_context(nc.sbuf_tensor(tag + "zeros", [128, 128], F32))
            ones1 = st.enter_context(nc.sbuf_tensor(tag + "ones1", [1, 128], F32))
            bg = st.enter_context(nc.sbuf_tensor(tag + "bg", [1, D], F32))
            g2 = st.enter_context(nc.sbuf_tensor(tag + "g2", [128, D], F32))
            b_rep = Buf()
            b_g2 = Buf()
            S.run("sp", lambda e: e.dma_start(out=bg[:], in_=bgate_ap), writes=[b_rep], dma="s")
            S.run("dve", lambda e: e.memset(zeros[:], 0.0), writes=[b_rep])
            S.run("dve", lambda e: e.memset(ones1[:], 1.0), writes=[b_rep])
            for k in range(KC):
                S.run("dve", lambda e, k=k: e.tensor_scalar(out=csrep[:, k, :], in0=zeros[:], scalar1=csT[:, k:k + 1],
                                                             scalar2=None, op0=ALU.add), reads=[b_cs, b_rep], writes=[b_rep])
        colbank = 0
        for jb in range(nblk):
            wt, wb, wn = ring.next()
            S.run("sp", lambda e, wt=wt, jb=jb: e.dma_start(
                out=wt[:], in_=wm[:, jb * 128:(jb + 1) * 128].rearrange("(k p) c -> p k c", p=128)),
                writes=[wb], dma=wn)
            if jb < 64:
                fns = [lambda e, wt=wt, k=k, jb=jb: e.matmul(ps[colbank][:, jb:jb + 1], wt[:, k, :], csT[:, k:k + 1],
                                                             start=(k == 0), stop=(k == KC - 1)) for k in range(KC)]
                S.run("pe", fns, reads=[wb, b_cs], writes=[pb[colbank]])
            else:
                gi = jb - 64
                bank = 1 + (gi % 4)
                fns = [lambda e, wt=wt, k=k, bank=bank: e.matmul(ps[bank][:, 0:128], csrep[:, k, :], wt[:, k, :],
                                                                 start=(k == 0), stop=False) for k in range(KC)]
                fns.append(lambda e, gi=gi, bank=bank: e.matmul(ps[bank][:, 0:128], ones1[0:1, :],
                                                                bg[0:1, gi * 128:(gi + 1) * 128], start=False, stop=True))
                S.run("pe", fns, reads=[wb, b_rep], writes=[pb[bank]])
                S.run("dve", lambda e, gi=gi, bank=bank: e.tensor_copy(out=g2[:, gi * 128:(gi + 1) * 128], in_=ps[bank][:, 0:128]),
                      reads=[pb[bank]], writes=[b_g2])
        b_o = Buf()
        S.run("dve", lambda e: e.tensor_tensor(out=modc[:], in0=ps[colbank][:, 0:64], in1=bmT[:], op=ALU.add),
              reads=[pb[colbank], b_small], writes=[b_o])
        S.run("dve", lambda e: e.tensor_copy(out=bt[:], in_=modc[:, 0:32]), reads=[b_o], writes=[b_o])
        S.run("dve", lambda e: e.scalar_tensor_tensor(out=at[:], in0=modc[:, 32:64], scalar=1.0, in1=gT[:],
                                                      op0=ALU.add, op1=ALU.mult), reads=[b_o], writes=[b_o])
        S.run("sp", lambda e: e.dma_start(out=at_out, in_=at[:]), reads=[b_o], dma="o")
        S.run("sp", lambda e: e.dma_start(out=bt_out, in_=bt[:]), reads=[b_o], dma="o")
        if has_gate:
            S.run("sp", lambda e: e.dma_start(out=g2_out, in_=g2[:]), reads=[b_g2], dma="o")
        S.emit()


class Common:
    pass


def emit_prologue(S, C, xsrc_tile_ap, m, router=None):
    xt, xb = C.xt, C.xt_b
    S.run("sp", lambda e: e.dma_start(out=xt[:], in_=xsrc_tile_ap), writes=[xb], dma="xt")
    S.run("act", lambda e: e.activation(out=C.junk_ap, in_=xt[:], func=AF.Square, accum_out=C.ss[:]),
          reads=[xb], writes=[C.junk_b, C.ss_b])
    S.run("act", lambda e: e.activation(out=C.ss[:], in_=C.ss[:], func=AF.Sqrt, bias=C.epst[:], scale=1.0 / D),
          reads=[C.ss_b, C.const_b], writes=[C.ss_b])
    S.run("dve", lambda e: e.reciprocal(out=C.rstd[:], in_=C.ss[:]), reads=[C.ss_b], writes=[C.rstd_b])
    S.run("act", lambda e: e.activation(out=xt[:], in_=xt[:], func=AF.Copy, scale=C.rstd[:]),
          reads=[xb, C.rstd_b], writes=[xb])
    for q in range(8):
        bank = C.tp_banks[q % len(C.tp_banks)]
        fns = [lambda e, q=q, i=i, bank=bank: e.transpose(C.ps[bank][:, i * 128:(i + 1) * 128],
                                                          xt[:, (q * 4 + i) * 128:(q * 4 + i + 1) * 128], C.ident[:])
               for i in range(4)]
        S.run("pe", fns, reads=[xb, C.const_b], writes=[C.pb[bank]])
        for i in range(4):
            k = q * 4 + i
            S.run("act", lambda e, k=k, i=i, bank=bank: e.activation(
                out=C.hT[:, k, m * 128:(m + 1) * 128], in_=C.ps[bank][:, i * 128:(i + 1) * 128],
                func=AF.Identity, scale=C.at[:, k:k + 1], bias=C.bt[:, k:k + 1]),
                reads=[C.pb[bank], C.mod_b], writes=[C.hT_b])


def load_common(st, nc, S, tag, at_d, bt_d, g2_d, ident_d, need_g2=True, own_junk=True, npsum=8):
    C = Common()
    C.ps, C.pb = psum_banks(st, nc, tag, npsum)
    C.xt = st.enter_context(nc.sbuf_tensor(tag + "xt", [128, D], F32))
    C.xt_b = Buf()
    if own_junk:
        C.junk = st.enter_context(nc.sbuf_tensor(tag + "junk", [128, D], BF16))
        C.junk_ap = C.junk[:]
        C.junk_b = Buf()
    C.ss = st.enter_context(nc.sbuf_tensor(tag + "ss", [128, 1], F32))
    C.ss_b = Buf()
    C.rstd = st.enter_context(nc.sbuf_tensor(tag + "rstd", [128, 1], F32))
    C.rstd_b = Buf()
    C.epst = st.enter_context(nc.sbuf_tensor(tag + "eps", [128, 1], F32))
    C.ident = st.enter_context(nc.sbuf_tensor(tag + "ident", [128, 128], F32))
    C.at = st.enter_context(nc.sbuf_tensor(tag + "at", [128, KC], F32))
    C.bt = st.enter_context(nc.sbuf_tensor(tag + "bt", [128, KC], F32))
    C.const_b = Buf()
    C.mod_b = Buf()
    S.run("dve", lambda e: e.memset(C.epst[:], EPS), writes=[C.const_b])
    S.run("sp", lambda e: e.dma_start(out=C.ident[:], in_=ident_d), writes=[C.const_b], dma="c")
    S.run("sp", lambda e: e.dma_start(out=C.at[:], in_=at_d), writes=[C.mod_b], dma="c")
    S.run("sp", lambda e: e.dma_start(out=C.bt[:], in_=bt_d), writes=[C.mod_b], dma="c")
    if need_g2:
        C.g2 = st.enter_context(nc.sbuf_tensor(tag + "g2", [128, D], F32))
        C.g2_b = Buf()
        S.run("sp", lambda e: e.dma_start(out=C.g2[:], in_=g2_d), writes=[C.g2_b], dma="c")
    C.hT = st.enter_context(nc.sbuf_tensor(tag + "hT", [128, KC, 512], BF16))
    C.hT_b = Buf()
    C.tp_banks = [6, 7]
    return C


def groups_of(ntiles, g=4):
    out = []
    s = 0
    while s < ntiles:
        out.append((s, min(g, ntiles - s)))
        s += g
    return out


def stage_conv(nc, tag, ntiles, x_src, x_dst, w_in, w_out, wconvT_d, flag_d, at_d, bt_d, g2_d, ident_d):
    with ExitStack() as st:
        S = Sched(nc, tag)
        C = load_common(st, nc, S, tag, at_d, bt_d, g2_d, ident_d)
        ps, pb = C.ps, C.pb
        gT = st.enter_context(nc.sbuf_tensor(tag + "gT", [128, KC, 512], BF16))
        gT_b = Buf()
        wring = Ring(st, nc, tag + "w", 3, [128, KC, 256], BF16)
        wc = st.enter_context(nc.sbuf_tensor(tag + "wc", [128, 3, KC], F32))
        flag = st.enter_context(nc.sbuf_tensor(tag + "flag", [128, 1], F32))
        carry = st.enter_context(nc.sbuf_tensor(tag + "carry", [128, KC, 2], F32))
        carry_b = Buf()
        vring = Ring(st, nc, tag + "v", 2, [128, 514], F32)
        cring = Ring(st, nc, tag + "c", 2, [128, 512], F32)
        aring = Ring(st, nc, tag + "a", 2, [128, 512], F32)
        xring = Ring(st, nc, tag + "xb", 3, [128, 512], F32)
        oring = Ring(st, nc, tag + "ob", 3, [128, 512], F32)
        S.run("sp", lambda e: e.dma_start(out=wc[:], in_=wconvT_d), writes=[C.const_b], dma="c")
        S.run("sp", lambda e: e.dma_start(out=flag[:], in_=flag_d), writes=[C.const_b], dma="c")
        S.run("dve", lambda e: e.memset(carry[:], 0.0), writes=[carry_b])
        for (t0, gt) in groups_of(ntiles):
            T = gt * 128
            for m in range(gt):
                emit_prologue(S, C, x_src[(t0 + m) * 128:(t0 + m + 1) * 128, :], m)
            for i in range(KC):
                banks = (0, 1, 2) if i % 2 == 0 else (3, 4, 5)
                for part in range(3):
                    if part % 2 == 0 or True:
                        wt, wb, wn = wring.next()
                        c0 = i * 384 + part * 128
                        S.run("pool", lambda e, wt=wt, c0=c0: e.dma_start(
                            out=wt[:, :, 0:128], in_=w_in[:, c0:c0 + 128].rearrange("(k p) c -> p k c", p=128)),
                            writes=[wb], dma=wn)
                    bank = banks[part]
                    fns = [lambda e, wt=wt, k=k, bank=bank, T=T: e.matmul(ps[bank][:, 0:T], wt[:, k, 0:128], C.hT[:, k, 0:T],
                                                                            start=(k == 0), stop=(k == KC - 1)) for k in range(KC)]
                    S.run("pe", fns, reads=[wb, C.hT_b], writes=[pb[bank]])
                vt, vb, _ = vring.next()
                ct, cb, _ = cring.next()
                at_, ab, _ = aring.next()
                S.run("act", lambda e, ct=ct, T=T, bk=banks[1]: e.copy(out=ct[:, 0:T], in_=ps[bk][:, 0:T]),
                      reads=[pb[banks[1]]], writes=[cb])
                S.run("dve", lambda e, vt=vt, i=i: e.tensor_copy(out=vt[:, 0:2], in_=carry[:, i, :]),
                      reads=[carry_b], writes=[vb])
                S.run("dve", lambda e, vt=vt, ct=ct, T=T, bk=banks[2]: e.tensor_tensor(
                    out=vt[:, 2:2 + T], in0=ct[:, 0:T], in1=ps[bk][:, 0:T], op=ALU.mult),
                    reads=[cb, pb[banks[2]]], writes=[vb])
                if t0 == 0:
                    S.run("dve", lambda e, vt=vt: e.tensor_scalar(out=vt[:, 2 + 254:2 + 256], in0=vt[:, 2 + 254:2 + 256],
                                                                   scalar1=flag[:, 0:1], scalar2=None, op0=ALU.mult),
                          reads=[vb, C.const_b], writes=[vb])
                S.run("dve", lambda e, vt=vt, i=i, T=T: e.tensor_copy(out=carry[:, i, :], in_=vt[:, T:T + 2]),
                      reads=[vb], writes=[carry_b])
                S.run("dve", lambda e, vt=vt, at_=at_, i=i, T=T: e.tensor_scalar(
                    out=at_[:, 0:T], in0=vt[:, 0:T], scalar1=wc[:, 0, i:i + 1], scalar2=None, op0=ALU.mult),
                    reads=[vb, C.const_b], writes=[ab])
                S.run("dve", lambda e, vt=vt, at_=at_, i=i, T=T: e.scalar_tensor_tensor(
                    out=at_[:, 0:T], in0=vt[:, 1:1 + T], scalar=wc[:, 1, i:i + 1], in1=at_[:, 0:T], op0=ALU.mult, op1=ALU.add),
                    reads=[vb, ab], writes=[ab])
                S.run("dve", lambda e, vt=vt, at_=at_, i=i, T=T: e.scalar_tensor_tensor(
                    out=at_[:, 0:T], in0=vt[:, 2:2 + T], scalar=wc[:, 2, i:i + 1], in1=at_[:, 0:T], op0=ALU.mult, op1=ALU.add),
                    reads=[vb, ab], writes=[ab])
                S.run("dve", lambda e, at_=at_, i=i, T=T, bk=banks[0]: e.tensor_tensor(
                    out=gT[:, i, 0:T], in0=at_[:, 0:T], in1=ps[bk][:, 0:T], op=ALU.mult),
                    reads=[ab, pb[banks[0]]], writes=[gT_b])
            for nb in range(16):
                wt, wb, wn = wring.next()
                S.run("pool", lambda e, wt=wt, nb=nb: e.dma_start(
                    out=wt[:], in_=w_out[:, nb * 256:(nb + 1) * 256].rearrange("(k p) c -> p k c", p=128)),
                    writes=[wb], dma=wn)
                for m in range(gt):
                    bank = (nb * gt + m) % 6
                    fns = [lambda e, wt=wt, k=k, m=m, bank=bank: e.matmul(ps[bank][:, 0:256], gT[:, k, m * 128:(m + 1) * 128], wt[:, k, :],
                                                                            start=(k == 0), stop=(k == KC - 1)) for k in range(KC)]
                    S.run("pe", fns, reads=[wb, gT_b], writes=[pb[bank]])
                    xb_t, xb_b, xn = xring.next()
                    ob_t, ob_b, on = oring.next()
                    r0 = (t0 + m) * 128
                    S.run("sp", lambda e, xb_t=xb_t, r0=r0, nb=nb: e.dma_start(out=xb_t[:, 0:256], in_=x_src[r0:r0 + 128, nb * 256:(nb + 1) * 256]),
                          writes=[xb_b], dma=xn)
                    S.run("dve", lambda e, ob_t=ob_t, bank=bank, nb=nb: e.tensor_tensor(
                        out=ob_t[:, 0:256], in0=ps[bank][:, 0:256], in1=C.g2[:, nb * 256:(nb + 1) * 256], op=ALU.mult),
                        reads=[pb[bank], C.g2_b], writes=[ob_b])
                    S.run("dve", lambda e, ob_t=ob_t, xb_t=xb_t: e.tensor_tensor(
                        out=ob_t[:, 0:256], in0=ob_t[:, 0:256], in1=xb_t[:, 0:256], op=ALU.add),
                        reads=[xb_b, ob_b], writes=[ob_b])
                    S.run("act", lambda e, ob_t=ob_t, r0=r0, nb=nb: e.dma_start(out=x_dst[r0:r0 + 128, nb * 256:(nb + 1) * 256], in_=ob_t[:, 0:256]),
                          reads=[ob_b], dma=on)
        S.emit()


def stage_moe(nc, tag, tiles, E, x_src, x_dst, w_gu, w_dn, wr_d, br_d, bguT_d, bdn_d, at_d, bt_d, g2_d, ident_d, dst_off=0):
    with ExitStack() as st:
        S = Sched(nc, tag)
        C = load_common(st, nc, S, tag, at_d, bt_d, g2_d, ident_d, own_junk=False)
        ps, pb = C.ps, C.pb
        yacc = st.enter_context(nc.sbuf_tensor(tag + "yacc", [128, 4, D], F32))
        yacc_b = [Buf() for _ in range(4)]
        C.junk_ap = yacc[:, 3, :]
        C.junk_b = yacc_b[3]
        aring = Ring(st, nc, tag + "A", 2, [128, KC, 256], BF16)
        bring = Ring(st, nc, tag + "B", 4, [128, 4, 512], BF16)
        bgu = st.enter_context(nc.sbuf_tensor(tag + "bgu", [128, E, 8], F32))
        bdnring = Ring(st, nc, tag + "bdn", 2, [E, 512], F32)
        G = st.enter_context(nc.sbuf_tensor(tag + "G", [128, 4, E], F32))
        GT = st.enter_context(nc.sbuf_tensor(tag + "GT", [E, 4, 128], F32))
        G_b = Buf()
        lg = st.enter_context(nc.sbuf_tensor(tag + "lg", [128, E], F32))
        ex = st.enter_context(nc.sbuf_tensor(tag + "ex", [128, E], F32))
        mk = st.enter_context(nc.sbuf_tensor(tag + "mk", [128, E], F32))
        m8 = st.enter_context(nc.sbuf_tensor(tag + "m8", [128, 8], F32))
        sm = st.enter_context(nc.sbuf_tensor(tag + "sm", [128, 2], F32))
        r_b = Buf()
        actT = [st.enter_context(nc.sbuf_tensor(tag + f"act{j}", [128, 512], BF16)) for j in range(4)]
        act_b = [Buf() for _ in range(4)]
        t1r = Ring(st, nc, tag + "t1", 2, [128, 512], F32)
        t2r = Ring(st, nc, tag + "t2", 2, [128, 512], F32)
        sgr = Ring(st, nc, tag + "sg", 2, [128, 512], F32)
        S.run("sp", lambda e: e.dma_start(out=bgu[:], in_=bguT_d), writes=[C.const_b], dma="c")
        wrb = st.enter_context(nc.sbuf_tensor(tag + "wrb", [128, KC, E], BF16))
        brb = st.enter_context(nc.sbuf_tensor(tag + "brb", [1, E], BF16))
        ones1b = st.enter_context(nc.sbuf_tensor(tag + "ones1b", [1, 128], BF16))
        S.run("dve", lambda e: e.memset(ones1b[:], 1.0), writes=[C.const_b])
        S.run("pool", lambda e: e.dma_start(out=wrb[:], in_=wr_d), writes=[C.const_b], dma="cw")
        S.run("pool", lambda e: e.dma_start(out=brb[:], in_=br_d), writes=[C.const_b], dma="cw")
        RB = 5
        for (g0, gt) in groups_of(len(tiles)):
            T = gt * 128
            for m in range(gt):
                tix = tiles[g0 + m]
                emit_prologue(S, C, x_src[tix * 128:(tix + 1) * 128, :], m)
                fns = [lambda e, k=k, m=m: e.matmul(ps[RB][:, 0:E], C.hT[:, k, m * 128:(m + 1) * 128], wrb[:, k, :],
                                                    start=(k == 0), stop=False) for k in range(KC)]
                fns.append(lambda e: e.matmul(ps[RB][:, 0:E], ones1b[0:1, :], brb[0:1, :], start=False, stop=True))
                S.run("pe", fns, reads=[C.const_b, C.hT_b], writes=[pb[RB]])
                S.run("dve", lambda e: e.tensor_copy(out=lg[:], in_=ps[RB][:, 0:E]), reads=[pb[RB]], writes=[r_b])
                S.run("dve", lambda e: e.max(out=m8[:], in_=lg[:]), reads=[r_b], writes=[r_b])
                S.run("dve", lambda e: e.tensor_scalar(out=mk[:], in0=lg[:], scalar1=m8[:, 3:4], scalar2=None, op0=ALU.is_ge),
                      reads=[r_b], writes=[r_b])
                S.run("dve", lambda e: e.tensor_scalar(out=sm[:, 0:1], in0=m8[:, 0:1], scalar1=-1.0, scalar2=None, op0=ALU.mult),
                      reads=[r_b], writes=[r_b])
                S.run("act", lambda e: e.activation(out=ex[:], in_=lg[:], func=AF.Exp, bias=sm[:, 0:1], scale=1.0),
                      reads=[r_b], writes=[r_b])
                S.run("dve", lambda e: e.tensor_tensor(out=ex[:], in0=ex[:], in1=mk[:], op=ALU.mult), reads=[r_b], writes=[r_b])
                S.run("dve", lambda e: e.reduce_sum(out=sm[:, 1:2], in_=ex[:], axis=mybir.AxisListType.X), reads=[r_b], writes=[r_b])
                S.run("dve", lambda e: e.reciprocal(out=sm[:, 1:2], in_=sm[:, 1:2]), reads=[r_b], writes=[r_b])
                S.run("dve", lambda e, m=m: e.tensor_scalar(out=G[:, m, :], in0=ex[:], scalar1=sm[:, 1:2], scalar2=None, op0=ALU.mult),
                      reads=[r_b], writes=[G_b])
                S.run("pe", lambda e, m=m: e.transpose(ps[RB][0:E, 128:256], G[:, m, :], C.ident[:]),
                      reads=[G_b, C.const_b], writes=[pb[RB]])
                S.run("dve", lambda e, m=m: e.tensor_copy(out=GT[:, m, :], in_=ps[RB][0:E, 128:256]), reads=[pb[RB]], writes=[G_b])
            STOP = 9
            for n in range(8):
                bt_, bb_, bn_ = bdnring.next()
                S.run("sp", lambda e, bt_=bt_, n=n: e.dma_start(out=bt_[:], in_=bdn_d[:, n * 512:(n + 1) * 512]), writes=[bb_], dma=bn_)
                for m in range(gt):
                    bank = (n * gt + m) % 2 + 6
                    S.run("pe", lambda e, bt_=bt_, m=m, bank=bank: e.matmul(ps[bank][:, :], GT[:, m, :], bt_[:], start=True, stop=True),
                          reads=[G_b, bb_], writes=[pb[bank]])
                    S.run("act", lambda e, m=m, n=n, bank=bank: e.copy(out=yacc[:, m, n * 512:(n + 1) * 512], in_=ps[bank][:, :]),
                          reads=[pb[bank]], writes=[yacc_b[m]])
            for ex_ in range(E if STOP > 2 else 0):
                for j in range(4):
                    wt, wb, wn = aring.next()
                    r0 = ex_ * D
                    S.run("pool", lambda e, wt=wt, ex_=ex_, j=j: e.dma_start(
                        out=wt[:], in_=w_gu(ex_)[:, j * 256:(j + 1) * 256].rearrange("(k p) c -> p k c", p=128)),
                        writes=[wb], dma=wn)
                    bg_, bl_ = (0, 1) if j % 2 == 0 else (2, 3)
                    for half, bank in ((0, bg_), (1, bl_)):
                        fns = [lambda e, wt=wt, k=k, half=half, bank=bank, T=T: e.matmul(
                            ps[bank][:, 0:T], wt[:, k, half * 128:(half + 1) * 128], C.hT[:, k, 0:T],
                            start=(k == 0), stop=(k == KC - 1)) for k in range(KC)]
                        S.run("pe", fns, reads=[wb, C.hT_b], writes=[pb[bank]])
                    t1, t1b, _ = t1r.next()
                    t2, t2b, _ = t2r.next()
                    sg, sgb, _ = sgr.next()
                    S.run("dve", lambda e, t1=t1, T=T, ex_=ex_, j=j, bank=bg_: e.tensor_scalar(
                        out=t1[:, 0:T], in0=ps[bank][:, 0:T], scalar1=bgu[:, ex_, j:j + 1], scalar2=7.0, op0=ALU.add, op1=ALU.min),
                        reads=[pb[bg_], C.const_b], writes=[t1b])
                    S.run("act", lambda e, sg=sg, t1=t1, T=T: e.activation(out=sg[:, 0:T], in_=t1[:, 0:T], func=AF.Sigmoid, scale=1.702),
                          reads=[t1b], writes=[sgb])
                    S.run("dve", lambda e, t2=t2, T=T, ex_=ex_, j=j, bank=bl_: e.tensor_scalar(
                        out=t2[:, 0:T], in0=ps[bank][:, 0:T], scalar1=bgu[:, ex_, 4 + j:5 + j], scalar2=7.0, op0=ALU.add, op1=ALU.min),
                        reads=[pb[bl_], C.const_b], writes=[t2b])
                    S.run("dve", lambda e, t2=t2, T=T: e.tensor_scalar(
                        out=t2[:, 0:T], in0=t2[:, 0:T], scalar1=-7.0, scalar2=1.0, op0=ALU.max, op1=ALU.add),
                        reads=[t2b], writes=[t2b])
                    S.run("dve", lambda e, t1=t1, sg=sg, T=T: e.tensor_tensor(out=t1[:, 0:T], in0=t1[:, 0:T], in1=sg[:, 0:T], op=ALU.mult),
                          reads=[t1b, sgb], writes=[t1b])
                    S.run("dve", lambda e, t1=t1, t2=t2, T=T, j=j: e.tensor_tensor(out=actT[j][:, 0:T], in0=t1[:, 0:T], in1=t2[:, 0:T], op=ALU.mult),
                          reads=[t1b, t2b], writes=[act_b[j]])
                for n in range(8):
                    wt, wb, wn = bring.next()
                    r0 = ex_ * FH
                    S.run("pool", lambda e, wt=wt, ex_=ex_, n=n: e.dma_start(
                        out=wt[:], in_=w_dn(ex_)[:, n * 512:(n + 1) * 512].rearrange("(j p) c -> p j c", p=128)),
                        writes=[wb], dma=wn)
                    for m in range(gt):
                        bank = (n * gt + m) % 2 + 6
                        fns = [lambda e, wt=wt, j=j, m=m, bank=bank: e.matmul(ps[bank][:, :], actT[j][:, m * 128:(m + 1) * 128], wt[:, j, :],
                                                                                start=(j == 0), stop=(j == 3)) for j in range(4)]
                        S.run("pe", fns, reads=[wb] + act_b, writes=[pb[bank]])
                        S.run("dve", lambda e, m=m, n=n, bank=bank, ex_=ex_: e.scalar_tensor_tensor(
                            out=yacc[:, m, n * 512:(n + 1) * 512], in0=ps[bank][:, :], scalar=G[:, m, ex_:ex_ + 1],
                            in1=yacc[:, m, n * 512:(n + 1) * 512], op0=ALU.mult, op1=ALU.add),
                            reads=[pb[bank], G_b, yacc_b[m]], writes=[yacc_b[m]])
            for m in range(gt):
                tix = tiles[g0 + m]
                S.run("sp", lambda e, tix=tix: e.dma_start(out=C.xt[:], in_=x_src[tix * 128:(tix + 1) * 128, :]), writes=[C.xt_b], dma="xt")
                S.run("dve", lambda e, m=m: e.tensor_tensor(out=yacc[:, m, :], in0=yacc[:, m, :], in1=C.g2[:], op=ALU.mult),
                      reads=[C.g2_b, yacc_b[m]], writes=[yacc_b[m]])
                S.run("dve", lambda e, m=m: e.tensor_tensor(out=yacc[:, m, :], in0=yacc[:, m, :], in1=C.xt[:], op=ALU.add),
                      reads=[C.xt_b, yacc_b[m]], writes=[yacc_b[m]])
                S.run("act", lambda e, m=m, tix=tix: e.dma_start(out=x_dst[(tix - dst_off) * 128:(tix - dst_off + 1) * 128, :], in_=yacc[:, m, :]),
                      reads=[yacc_b[m]], writes=[yacc_b[m]], dma="yo")
        S.emit()


def colT(v, nchunk):
    v = np.asarray(v)
    lead = v.shape[:-1]
    return np.ascontiguousarray(np.moveaxis(v.reshape(*lead, nchunk, 128), -1, 0))


def prep_weights(inp, E, L=2):
    W = {}
    mods = [inp["w_mod_mix"][0], inp["w_mod_ffn"][0], inp["w_mod_kv"], inp["w_mod_mix"][1], inp["w_mod_ffn"][1]]
    bmods = [inp["b_mod_mix"][0], inp["b_mod_ffn"][0], inp["b_mod_kv"], inp["b_mod_mix"][1], inp["b_mod_ffn"][1]]
    gs = [inp["g_mix"][0], inp["g_ffn"][0], inp["g_kv"], inp["g_mix"][1], inp["g_ffn"][1]]
    for i in range(5):
        W[f"wmod{i}"] = np.ascontiguousarray(mods[i])
        W[f"bmT{i}"] = colT(bmods[i][:2 * D], 64)
        W[f"gT{i}"] = colT(gs[i], KC)
        if mods[i].shape[1] == 3 * D:
            W[f"bg{i}"] = np.ascontiguousarray(bmods[i][2 * D:].reshape(1, D))
    w_in = inp["w_a_in"][0].reshape(D, 3, KC, 128).transpose(0, 2, 1, 3).reshape(D, 3 * D)
    W["w_in"] = np.ascontiguousarray(w_in)
    W["w_out"] = np.ascontiguousarray(inp["w_a_out"][0])
    W["wconvT"] = colT(inp["w_a_conv"][0], KC)
    wgu = inp["w_gu"][:, :E]
    wgu = wgu.reshape(L, E, D, 4, 128, 2).transpose(0, 1, 2, 3, 5, 4).reshape(L, E * D, 1024)
    wdn = inp["w_dn"][:, :E].reshape(L, E * FH, D)
    for l in range(L):
        W[f"w_gu{l}"] = np.ascontiguousarray(wgu[l])
        W[f"w_dn{l}"] = np.ascontiguousarray(wdn[l])
        W[f"wr{l}"] = np.ascontiguousarray(inp["w_router"][l][:, :E].reshape(KC, 128, E).transpose(1, 0, 2))
        W[f"br{l}"] = np.ascontiguousarray(inp["b_router"][l][:E].reshape(1, E))
        bgu = inp["b_gu"][l][:E]
        glu = bgu[:, 0::2].reshape(E, 4, 128)
        lin = bgu[:, 1::2].reshape(E, 4, 128)
        W[f"bguT{l}"] = np.ascontiguousarray(np.concatenate([glu, lin], axis=1).transpose(2, 0, 1))
        W[f"bdn{l}"] = np.ascontiguousarray(inp["b_dn"][l][:E])
    W["ident"] = np.eye(128, dtype=np.float32)
    return W


SHARDED = ["wmod0", "wmod1", "wmod2", "wmod3", "wmod4", "w_in", "w_out", "w_gu0", "w_dn0", "w_gu1", "w_dn1",
           "w_kv", "w_q", "w_o"]


def qk_norm(S, C, nc_t, pbank, pb_, src_ps, width, bias_col, gcol, scale, out_ap, tmp):
    kf, sq, rr, b = tmp
    S.run("act", lambda e: e.activation(out=kf[:, 0:width], in_=src_ps, func=AF.Identity, bias=bias_col, scale=1.0),
          reads=[pb_[0], C.const_b], writes=[b])
    S.run("act", lambda e: e.activation(out=sq[:, 0:width], in_=kf[:, 0:width], func=AF.Square), reads=[b], writes=[b])
    S.run("pe", lambda e: e.matmul(C.ps[pbank][:, 0:width], C.bdiag[:], sq[:, 0:width], start=True, stop=True),
          reads=[b, C.const_b], writes=[C.pb[pbank]])
    S.run("act", lambda e: e.activation(out=rr[:, 0:width], in_=C.ps[pbank][:, 0:width], func=AF.Sqrt, bias=C.epst[:], scale=1.0 / HD),
          reads=[C.pb[pbank], C.const_b], writes=[b])
    S.run("dve", lambda e: e.reciprocal(out=rr[:, 0:width], in_=rr[:, 0:width]), reads=[b], writes=[b])
    S.run("dve", lambda e: e.tensor_tensor(out=kf[:, 0:width], in0=kf[:, 0:width], in1=rr[:, 0:width], op=ALU.mult), reads=[b], writes=[b])
    return S.run("dve", lambda e: e.tensor_scalar(out=out_ap, in0=kf[:, 0:width], scalar1=gcol, scalar2=float(scale), op0=ALU.mult, op1=ALU.mult),
                 reads=[b, C.const_b], writes=[b])


def stage_kv(nc, tag, tiles, x_src, w_k, w_ksw, w_v, bkT_d, bkswT_d, bv_d, gkT_d, bdiag_d, at_d, bt_d, ident_d, kT_d, kTsw_d, va_d):
    with ExitStack() as st:
        S = Sched(nc, tag)
        C = load_common(st, nc, S, tag, at_d, bt_d, None, ident_d, need_g2=False)
        ps, pb = C.ps, C.pb
        C.bdiag = st.enter_context(nc.sbuf_tensor(tag + "bdiag", [128, 128], F32))
        wk = st.enter_context(nc.sbuf_tensor(tag + "wk", [128, KC, 512], BF16))
        wksw = st.enter_context(nc.sbuf_tensor(tag + "wksw", [128, KC, 512], BF16))
        wv = st.enter_context(nc.sbuf_tensor(tag + "wv", [128, KC, 512], BF16))
        bk = st.enter_context(nc.sbuf_tensor(tag + "bk", [128, 4], F32))
        bksw = st.enter_context(nc.sbuf_tensor(tag + "bksw", [128, 4], F32))
        gk = st.enter_context(nc.sbuf_tensor(tag + "gk", [128, 1], F32))
        bvf = st.enter_context(nc.sbuf_tensor(tag + "bvf", [1, 512], F32))
        bvb = st.enter_context(nc.sbuf_tensor(tag + "bvb", [1, 512], BF16))
        ones1b = st.enter_context(nc.sbuf_tensor(tag + "ones1b", [1, 128], BF16))
        kf = st.enter_context(nc.sbuf_tensor(tag + "kf", [128, 128], F32))
        sq = st.enter_context(nc.sbuf_tensor(tag + "sq", [128, 128], F32))
        rr = st.enter_context(nc.sbuf_tensor(tag + "rr", [128, 128], F32))
        tb = Buf()
        koring = Ring(st, nc, tag + "ko", 2, [128, 128], BF16)
        varing = Ring(st, nc, tag + "va", 2, [128, 8, 65], BF16)
        for (dst, src) in ((wk, w_k), (wksw, w_ksw), (wv, w_v)):
            S.run("pool", lambda e, dst=dst, src=src: e.dma_start(out=dst[:], in_=src.rearrange("(k p) c -> p k c", p=128)),
                  writes=[C.const_b], dma="w")
        for (dst, src) in ((bk, bkT_d), (bksw, bkswT_d), (gk, gkT_d), (bvf, bv_d), (C.bdiag, bdiag_d)):
            S.run("sp", lambda e, dst=dst, src=src: e.dma_start(out=dst[:], in_=src), writes=[C.const_b], dma="c")
        S.run("dve", lambda e: e.tensor_copy(out=bvb[:], in_=bvf[:]), reads=[C.const_b], writes=[C.const_b])
        S.run("dve", lambda e: e.memset(ones1b[:], 1.0), writes=[C.const_b])
        for tix in tiles:
            emit_prologue(S, C, x_src[tix * 128:(tix + 1) * 128, :], 0)
            for (wsel, bsel, dstd) in ((wk, bk, kT_d), (wksw, bksw, kTsw_d)):
                for cc in range(4):
                    bank = cc % 2
                    fns = [lambda e, wsel=wsel, k=k, cc=cc, bank=bank: e.matmul(ps[bank][:, 0:128], wsel[:, k, cc * 128:(cc + 1) * 128], C.hT[:, k, 0:128],
                                                                                  start=(k == 0), stop=(k == KC - 1)) for k in range(KC)]
                    S.run("pe", fns, reads=[C.const_b, C.hT_b], writes=[pb[bank]])
                    ko, kob, kon = koring.next()
                    S.run("dve", lambda e: e.tensor_copy(out=rr[:, 0:1], in_=rr[:, 0:1]), writes=[kob, tb])
                    qk_norm(S, C, nc, 2 + bank, [pb[bank]], ps[bank][:, 0:128], 128, bsel[:, cc:cc + 1], gk[:, 0:1], 1.0, ko[:], (kf, sq, rr, tb))
                    S.run("act", lambda e, ko=ko, dstd=dstd, tix=tix, cc=cc: e.dma_start(out=dstd[tix, cc], in_=ko[:]), reads=[tb], writes=[kob], dma=kon)
            fns = [lambda e, k=k: e.matmul(ps[4][:, :], C.hT[:, k, 0:128], wv[:, k, :], start=(k == 0), stop=False) for k in range(KC)]
            fns.append(lambda e: e.matmul(ps[4][:, :], ones1b[0:1, :], bvb[0:1, :], start=False, stop=True))
            S.run("pe", fns, reads=[C.const_b, C.hT_b], writes=[pb[4]])
            va, vab, van = varing.next()
            S.run("dve", lambda e, va=va: e.memset(va[:], 1.0), writes=[vab])
            S.run("dve", lambda e, va=va: e.tensor_copy(out=va[:, :, 0:64], in_=ps[4][:, :].rearrange("p (g d) -> p g d", d=64)),
                  reads=[pb[4]], writes=[vab])
            S.run("act", lambda e, va=va, tix=tix: e.dma_start(out=va_d[tix], in_=va[:]), reads=[vab], writes=[vab], dma=van)
        S.emit()


def stage_attn(nc, tag, tiles, x_src, x_dst, w_q, w_o, bqT_d, gqT_d, sinkb_d, bo_d, maskp_d, maskc_d, flag_d, bdiag_d,
               kT_d, kTsw_d, va_d, at_d, bt_d, g2_d, ident_d, identb_d):
    with ExitStack() as st:
        S = Sched(nc, tag)
        C = load_common(st, nc, S, tag, at_d, bt_d, g2_d, ident_d, npsum=6)
        ps, pb = C.ps, C.pb
        C.tp_banks = [4, 5]
        C.psb = [st.enter_context(nc.psum_tensor(f"{tag}psb{i}", [128, 1024], BF16)) for i in range(2)]
        pbb = [Buf(), Buf()]
        C.bdiag = st.enter_context(nc.sbuf_tensor(tag + "bdiag", [128, 128], F32))
        identb = st.enter_context(nc.sbuf_tensor(tag + "identb", [128, 128], BF16))
        bq = st.enter_context(nc.sbuf_tensor(tag + "bq", [128, KC], F32))
        gq = st.enter_context(nc.sbuf_tensor(tag + "gq", [128, 1], F32))
        sinke = st.enter_context(nc.sbuf_tensor(tag + "sinke", [128, NH], F32))
        bof = st.enter_context(nc.sbuf_tensor(tag + "bof", [1, D], F32))
        bob = st.enter_context(nc.sbuf_tensor(tag + "bob", [1, D], BF16))
        ones1b = st.enter_context(nc.sbuf_tensor(tag + "ones1b", [1, 128], BF16))
        maskp = st.enter_context(nc.sbuf_tensor(tag + "maskp", [128, 128], BF16))
        maskp0 = st.enter_context(nc.sbuf_tensor(tag + "maskp0", [128, 128], BF16))
        maskc = st.enter_context(nc.sbuf_tensor(tag + "maskc", [128, 128], BF16))
        mf = st.enter_context(nc.sbuf_tensor(tag + "mf", [128, 2, 128], F32))
        flag = st.enter_context(nc.sbuf_tensor(tag + "flag", [128, 1], F32))
        kst = st.enter_context(nc.sbuf_tensor(tag + "kst", [128, 5, 4, 128], BF16))
        ksw = st.enter_context(nc.sbuf_tensor(tag + "ksw", [128, 5, 4, 128], BF16))
        vst = st.enter_context(nc.sbuf_tensor(tag + "vst", [128, 5, 8, 65], BF16))
        kv_b = Buf()
        otok = st.enter_context(nc.sbuf_tensor(tag + "otok", [128, 4, D], BF16))
        otok_b = [Buf() for _ in range(4)]
        qn = st.enter_context(nc.sbuf_tensor(tag + "qn", [128, 512], BF16))
        qn_b = Buf()
        kf = st.enter_context(nc.sbuf_tensor(tag + "kf", [128, 512], F32))
        sq = st.enter_context(nc.sbuf_tensor(tag + "sq", [128, 512], F32))
        rr = st.enter_context(nc.sbuf_tensor(tag + "rr", [128, 512], F32))
        tb = Buf()
        wring = Ring(st, nc, tag + "w", 2, [128, KC, 256], BF16)
        pring = Ring(st, nc, tag + "p", 4, [128, 128], BF16)
        dring = Ring(st, nc, tag + "d", 2, [128, 1], F32)
        xring = Ring(st, nc, tag + "xb", 3, [128, 256], F32)
        oring = Ring(st, nc, tag + "ob", 3, [128, 256], F32)
        for (dst, src) in ((bq[:], bqT_d), (gq[:], gqT_d), (sinke[:], sinkb_d), (bof[:], bo_d), (mf[:, 0, :], maskp_d), (mf[:, 1, :], maskc_d),
                           (flag[:], flag_d), (C.bdiag[:], bdiag_d)):
            S.run("sp", lambda e, dst=dst, src=src: e.dma_start(out=dst, in_=src), writes=[C.const_b], dma="c")
        S.run("pool", lambda e: e.dma_start(out=identb[:], in_=identb_d), writes=[C.const_b], dma="w")
        S.run("act", lambda e: e.activation(out=sinke[:], in_=sinke[:], func=AF.Exp), reads=[C.const_b], writes=[C.const_b])
        S.run("dve", lambda e: e.tensor_copy(out=bob[:], in_=bof[:]), reads=[C.const_b], writes=[C.const_b])
        S.run("dve", lambda e: e.memset(ones1b[:], 1.0), writes=[C.const_b])
        S.run("dve", lambda e: e.tensor_copy(out=maskp[:], in_=mf[:, 0, :]), reads=[C.const_b], writes=[C.const_b])
        S.run("dve", lambda e: e.tensor_copy(out=maskc[:], in_=mf[:, 1, :]), reads=[C.const_b], writes=[C.const_b])
        S.run("dve", lambda e: e.tensor_scalar(out=maskp0[:], in0=mf[:, 0, :], scalar1=flag[:, 0:1], scalar2=None, op0=ALU.mult),
              reads=[C.const_b], writes=[C.const_b])
        first_tile = tiles[0]
        for (g0, gt) in groups_of(len(tiles)):
            T = gt * 128
            tl = tiles[g0:g0 + gt]
            for s in range(gt + 1):
                tix = tl[0] - 1 + s
                S.run("sp", lambda e, s=s, tix=tix: e.dma_start(out=kst[:, s], in_=kT_d[tix].rearrange("c p t -> p c t")), writes=[kv_b], dma="kv")
                S.run("sp", lambda e, s=s, tix=tix: e.dma_start(out=ksw[:, s], in_=kTsw_d[tix].rearrange("c p t -> p c t")), writes=[kv_b], dma="kv")
                S.run("sp", lambda e, s=s, tix=tix: e.dma_start(out=vst[:, s], in_=va_d[tix]), writes=[kv_b], dma="kv")
            for m in range(gt):
                emit_prologue(S, C, x_src[tl[m] * 128:(tl[m] + 1) * 128, :], m)
            for c in range(KC):
                if c % 2 == 0:
                    wt, wb, wn = wring.next()
                    S.run("pool", lambda e, wt=wt, c=c: e.dma_start(
                        out=wt[:], in_=w_q[:, c * 128:(c + 2) * 128].rearrange("(k p) n -> p k n", p=128)), writes=[wb], dma=wn)
                qb = c % 2
                fns = [lambda e, wt=wt, k=k, T=T, qb=qb, c=c: e.matmul(ps[qb][:, 0:T], wt[:, k, (c % 2) * 128:(c % 2 + 1) * 128], C.hT[:, k, 0:T],
                                                                        start=(k == 0), stop=(k == KC - 1)) for k in range(KC)]
                S.run("pe", fns, reads=[wb, C.hT_b], writes=[pb[qb]])
                S.run("dve", lambda e: e.tensor_copy(out=rr[:, 0:1], in_=rr[:, 0:1]), writes=[qn_b, tb])
                tq = qk_norm(S, C, nc, 2 + qb, [pb[qb]], ps[qb][:, 0:T], T, bq[:, c:c + 1], gq[:, 0:1], HD ** -0.5, qn[:, 0:T], (kf, sq, rr, tb))
                qn_b.did_write(tq)
                g = c // 4
                cc = g // 2
                for m in range(gt):
                    for h in range(2):
                        head = 2 * c + h
                        ksel = kst if (g % 2) == h else ksw
                        obank = 4 + (h % 2)
                        pts = []
                        for kb in range(2):
                            slot = m + kb
                            sbank = 2 + kb
                            S.run("pe", lambda e, ksel=ksel, slot=slot, h=h, m=m, sbank=sbank, cc=cc: e.matmul(
                                ps[sbank][:, 0:128], ksel[h * 64:(h + 1) * 64, slot, cc, :], qn[h * 64:(h + 1) * 64, m * 128:(m + 1) * 128],
                                start=True, stop=True), reads=[kv_b, qn_b], writes=[pb[sbank]])
                            pt, ptb, _ = pring.next()
                            S.run("act", lambda e, pt=pt, sbank=sbank: e.activation(out=pt[:], in_=ps[sbank][:, 0:128], func=AF.Exp),
                                  reads=[pb[sbank]], writes=[ptb])
                            mk_ = maskc if kb == 1 else (maskp0 if tl[m] == first_tile else maskp)
                            S.run("dve", lambda e, pt=pt, mk_=mk_: e.tensor_tensor(out=pt[:], in0=pt[:], in1=mk_[:], op=ALU.mult),
                                  reads=[ptb, C.const_b], writes=[ptb])
                            pts.append((pt, ptb, slot))
                        fns = [lambda e, pt=pt, slot=slot, i=i, obank=obank, g=g: e.matmul(ps[obank][:, 0:65], pt[:], vst[:, slot, g, :],
                                                                                           start=(i == 0), stop=(i == 1))
                               for i, (pt, ptb, slot) in enumerate(pts)]
                        S.run("pe", fns, reads=[pts[0][1], pts[1][1], kv_b], writes=[pb[obank]])
                        dn, dnb, _ = dring.next()
                        S.run("dve", lambda e, dn=dn, obank=obank, head=head: e.tensor_tensor(out=dn[:], in0=ps[obank][:, 64:65], in1=sinke[:, head:head + 1], op=ALU.add),
                              reads=[pb[obank], C.const_b], writes=[dnb])
                        S.run("dve", lambda e, dn=dn: e.reciprocal(out=dn[:], in_=dn[:]), reads=[dnb], writes=[dnb])
                        S.run("dve", lambda e, dn=dn, obank=obank, head=head, m=m: e.tensor_scalar(
                            out=otok[:, m, head * 64:(head + 1) * 64], in0=ps[obank][:, 0:64], scalar1=dn[:, 0:1], scalar2=None, op0=ALU.mult),
                            reads=[pb[obank], dnb], writes=[otok_b[m]])
            for m in range(gt):
                for q in range(8):
                    bank = q % 2
                    fns = [lambda e, q=q, i=i, bank=bank, m=m: e.transpose(C.psb[bank][:, i * 128:(i + 1) * 128],
                                                                            otok[:, m, (q * 4 + i) * 128:(q * 4 + i + 1) * 128], identb[:])
                           for i in range(4)]
                    S.run("pe", fns, reads=[otok_b[m], C.const_b], writes=[pbb[bank]])
                    S.run("act", lambda e, q=q, bank=bank, m=m: e.copy(out=C.hT[:, q * 4:(q + 1) * 4, m * 128:(m + 1) * 128],
                                                                          in_=C.psb[bank][:, 0:512].rearrange("p (i t) -> p i t", t=128)),
                          reads=[pbb[bank]], writes=[C.hT_b])
            for nb in range(16):
                wt, wb, wn = wring.next()
                S.run("pool", lambda e, wt=wt, nb=nb: e.dma_start(
                    out=wt[:], in_=w_o[:, nb * 256:(nb + 1) * 256].rearrange("(k p) c -> p k c", p=128)), writes=[wb], dma=wn)
                for m in range(gt):
                    bank = 2 + (nb * gt + m) % 4
                    fns = [lambda e, wt=wt, k=k, m=m, bank=bank: e.matmul(ps[bank][:, 0:256], C.hT[:, k, m * 128:(m + 1) * 128], wt[:, k, :],
                                                                            start=(k == 0), stop=False) for k in range(KC)]
                    fns.append(lambda e, bank=bank, nb=nb: e.matmul(ps[bank][:, 0:256], ones1b[0:1, :], bob[0:1, nb * 256:(nb + 1) * 256], start=False, stop=True))
                    S.run("pe", fns, reads=[wb, C.hT_b, C.const_b], writes=[pb[bank]])
                    xb_t, xb_b, xn = xring.next()
                    ob_t, ob_b, on = oring.next()
                    r0 = tl[m] * 128
                    S.run("sp", lambda e, xb_t=xb_t, r0=r0, nb=nb: e.dma_start(out=xb_t[:], in_=x_src[r0:r0 + 128, nb * 256:(nb + 1) * 256]), writes=[xb_b], dma=xn)
                    S.run("dve", lambda e, ob_t=ob_t, bank=bank, nb=nb: e.tensor_tensor(out=ob_t[:], in0=ps[bank][:, 0:256], in1=C.g2[:, nb * 256:(nb + 1) * 256], op=ALU.mult),
                          reads=[pb[bank], C.g2_b], writes=[ob_b])
                    S.run("dve", lambda e, ob_t=ob_t, xb_t=xb_t: e.tensor_tensor(out=ob_t[:], in0=ob_t[:], in1=xb_t[:], op=ALU.add), reads=[xb_b, ob_b], writes=[ob_b])
                    S.run("act", lambda e, ob_t=ob_t, r0=r0, nb=nb: e.dma_start(out=x_dst[r0:r0 + 128, nb * 256:(nb + 1) * 256], in_=ob_t[:]), reads=[ob_b], writes=[ob_b], dma=on)
        S.emit()


def prep_attn(inp):
    W = {}
    wkv = inp["w_kv"]
    wk = wkv[:, :512]
    sw = np.array([1, 0, 3, 2, 5, 4, 7, 6])
    W["w_k"] = np.ascontiguousarray(wk)
    W["w_ksw"] = np.ascontiguousarray(wk.reshape(D, 8, 64)[:, sw].reshape(D, 512))
    W["w_v"] = np.ascontiguousarray(wkv[:, 512:])
    bk = inp["b_kv"][:512]
    W["bkT"] = colT(bk, 4)
    W["bkswT"] = colT(bk.reshape(8, 64)[sw].reshape(512), 4)
    W["bv"] = np.ascontiguousarray(inp["b_kv"][512:].reshape(1, 512))
    W["gkT"] = np.ascontiguousarray(np.tile(inp["g_k"], 2).reshape(128, 1))
    W["gqT"] = np.ascontiguousarray(np.tile(inp["g_q"][0], 2).reshape(128, 1))
    bd = np.zeros((128, 128), np.float32)
    bd[:64, :64] = 1.0
    bd[64:, 64:] = 1.0
    W["bdiag"] = bd
    W["w_q"] = np.ascontiguousarray(inp["w_b_q"][0])
    W["w_o"] = np.ascontiguousarray(inp["w_b_o"][0])
    W["bqT"] = colT(inp["b_b_q"][0], KC)
    W["sinkb"] = np.ascontiguousarray(np.broadcast_to(inp["sinks"][0], (128, NH)))
    W["bo"] = np.ascontiguousarray(inp["b_b_o"][0].reshape(1, D))
    j = np.arange(128)[:, None]
    i = np.arange(128)[None, :]
    W["maskp"] = (j > i).astype(np.float32)
    W["maskc"] = (j <= i).astype(np.float32)
    W["identb"] = np.eye(128, dtype=np.float32)
    return W


NCORES = 8
NT_ALL = 18
E_FULL = 32
PIECES = {"wmod0": 1, "wmod1": 1, "wmod2": 1, "wmod3": 1, "wmod4": 1, "w_in": 1, "w_out": 1, "w_gu0": 4, "w_dn0": 2,
          "w_gu1": 4, "w_dn1": 2, "w_q": 1, "w_o": 1, "w_k": 1, "w_ksw": 1, "w_v": 1}
SMALL = ["bmT0", "bmT1", "bmT2", "bmT3", "bmT4", "gT0", "gT1", "gT2", "gT3", "gT4", "bg0", "bg1", "bg3", "bg4", "wconvT",
         "wr0", "wr1", "br0", "br1", "bguT0", "bguT1", "bdn0", "bdn1", "ident", "identb", "bkT", "bkswT", "bv", "gkT", "gqT",
         "bdiag", "bqT", "sinkb", "bo", "maskp", "maskc"]


def build_full(shapes):
    nc = bass.Bass("TRN2", target_bir_lowering=False)
    ext = {}
    for name, shp in shapes.items():
        ext[name] = nc.dram_tensor(name, list(shp), F32, kind="ExternalInput").ap()
    out = nc.dram_tensor("out", [(NT_ALL - 2) * 128, D], F32, kind="ExternalOutput").ap()
    full = {}
    for name, P in PIECES.items():
        if P == 1:
            full[name] = ext[name]
        else:
            full[name] = [ext[f"{name}_p{p}"] for p in range(P)]
    x1 = nc.dram_tensor("x1", [NT_ALL * 128, D], F32).ap()
    x2 = nc.dram_tensor("x2", [NT_ALL * 128, D], F32).ap()
    x3 = nc.dram_tensor("x3", [NT_ALL * 128, D], F32).ap()
    mods = []
    for i in range(5):
        at_d = nc.dram_tensor(f"at{i}", [128, KC], F32).ap()
        bt_d = nc.dram_tensor(f"bt{i}", [128, KC], F32).ap()
        has_gate = i != 2
        g2_d = nc.dram_tensor(f"g2{i}", [128, D], F32).ap() if has_gate else None
        stage_mod(nc, f"m{i}", full[f"wmod{i}"], 3 * D if has_gate else 2 * D, ext["cT"], ext[f"gT{i}"], ext[f"bmT{i}"],
                  ext[f"bg{i}"] if has_gate else None, at_d, bt_d, g2_d)
        mods.append((at_d, bt_d, g2_d))
    kT_d = nc.dram_tensor("kT_d", [NT_ALL, 4, 128, 128], BF16).ap()
    kTsw_d = nc.dram_tensor("kTsw_d", [NT_ALL, 4, 128, 128], BF16).ap()
    va_d = nc.dram_tensor("va_d", [NT_ALL, 128, 8, 65], BF16).ap()

    def gu_fn(l):
        epp = E_FULL // PIECES[f"w_gu{l}"]
        return lambda e: full[f"w_gu{l}"][e // epp][(e % epp) * D:(e % epp + 1) * D, :]

    def dn_fn(l):
        epp = E_FULL // PIECES[f"w_dn{l}"]
        return lambda e: full[f"w_dn{l}"][e // epp][(e % epp) * FH:(e % epp + 1) * FH, :]

    stage_conv(nc, "s1", NT_ALL, ext["x_in"], x1, full["w_in"], full["w_out"], ext["wconvT"], ext["flag"], *mods[0], ext["ident"])
    stage_moe(nc, "s2", list(range(1, NT_ALL)), E_FULL, x1, x2, gu_fn(0), dn_fn(0), ext["wr0"], ext["br0"], ext["bguT0"], ext["bdn0"],
              *mods[1], ext["ident"])
    stage_kv(nc, "s3", list(range(1, NT_ALL)), x2, full["w_k"], full["w_ksw"], full["w_v"], ext["bkT"], ext["bkswT"], ext["bv"], ext["gkT"],
             ext["bdiag"], mods[2][0], mods[2][1], ext["ident"], kT_d, kTsw_d, va_d)
    stage_attn(nc, "s4", list(range(2, NT_ALL)), x2, x3, full["w_q"], full["w_o"], ext["bqT"], ext["gqT"], ext["sinkb"], ext["bo"], ext["maskp"],
               ext["maskc"], ext["flag"], ext["bdiag"], kT_d, kTsw_d, va_d, *mods[3], ext["ident"], ext["identb"])
    stage_moe(nc, "s5", list(range(2, NT_ALL)), E_FULL, x3, out, gu_fn(1), dn_fn(1), ext["wr1"], ext["br1"], ext["bguT1"], ext["bdn1"],
              *mods[4], ext["ident"], dst_off=2)
    return nc


def shard_rows(Wm, r, P):
    rows, cols = Wm.shape
    return np.ascontiguousarray(Wm.reshape(P, NCORES, rows // (P * NCORES), cols)[:, r].reshape(-1, cols))


TEST_CORES = None


def kernel(**inputs):
    inp = {k: np.asarray(v) for k, v in inputs.items()}
    W = prep_weights(inp, E_FULL)
    W.update(prep_attn(inp))
    x = inp["x"]
    B, SEQ, _ = x.shape
    per = SEQ // 4
    shared = {}
    for name, P in PIECES.items():
        if P == 1:
            shared[name] = W[name]
        else:
            rows = W[name].shape[0] // P
            for p in range(P):
                shared[f"{name}_p{p}"] = np.ascontiguousarray(W[name][p * rows:(p + 1) * rows])
    for name in SMALL:
        shared[name] = W[name]
    ncores = NCORES if TEST_CORES is None else TEST_CORES
    in_maps = []
    for r in range(ncores):
        b, q = r // 4, r % 4
        m = dict(shared)
        xin = np.zeros((NT_ALL * 128, D), np.float32)
        if q > 0:
            xin[:256] = x[b, q * per - 256:q * per]
        xin[256:] = x[b, q * per:(q + 1) * per]
        m["x_in"] = xin
        m["cT"] = colT(inp["c"][b], KC)
        m["flag"] = np.full((128, 1), 1.0 if q > 0 else 0.0, np.float32)
        in_maps.append(m)
    shapes = {k: v.shape for k, v in in_maps[0].items()}
    nc = build_full(shapes)
    res = run_bass_kernel_spmd(nc, in_maps, core_ids=list(range(ncores)))
    out = np.zeros((B, SEQ, D), np.float32)
    for r in range(ncores):
        b, q = r // 4, r % 4
        out[b, q * per:(q + 1) * per] = res.results[r]["out"]
    return out
```

```python
from contextlib import ExitStack
import numpy as np
import concourse.bass as bass
import concourse.mybir as mybir
from concourse.bass_utils import run_bass_kernel_spmd

F32 = mybir.dt.float32
BF16 = mybir.dt.bfloat16
AF = mybir.ActivationFunctionType
ALU = mybir.AluOpType

D = 4096
KC = 32
FH = 512
HD = 64
NH = 64
NKV = 8
EPS = 1e-5


class Buf:
    def __init__(self):
        self.w = {}
        self.r = {}

    def rd_deps(self):
        return list(self.w.items())

    def wr_deps(self):
        return list(self.w.items()) + list(self.r.items())

    def did_read(self, t):
        self.r[t[0]] = max(self.r.get(t[0], 0), t[1])

    def did_write(self, t):
        self.w[t[0]] = max(self.w.get(t[0], 0), t[1])


class Sched:
    ENG = ("pe", "act", "dve", "pool", "sp")
    BLK = {"pe": "tensor", "act": "scalar", "dve": "vector", "pool": "gpsimd", "sp": "sync"}

    def __init__(self, nc, tag):
        self.nc = nc
        self.tag = tag
        self.q = {e: [] for e in self.ENG}
        self.cnt = {}
        self.seen = {e: {} for e in self.ENG}
        self.step = {}

    def _waits(self, eng, deps):
        need = {}
        for d in deps:
            if d is None:
                continue
            need[d[0]] = max(need.get(d[0], 0), d[1])
        out = []
        for k, v in need.items():
            if self.seen[eng].get(k, 0) < v:
                self.seen[eng][k] = v
                out.append((k, v))
        return out

    def op(self, eng, fn, deps=(), mark=True, dma=None, step=16):
        waits = self._waits(eng, deps)
        tok = None
        key = None
        if dma is not None:
            key = "D" + dma
            self.step[key] = step
            self.cnt[key] = self.cnt.get(key, 0) + step
            tok = (key, self.cnt[key])
        elif mark:
            key = "E" + eng
            self.step[key] = 1
            self.cnt[key] = self.cnt.get(key, 0) + 1
            tok = (key, self.cnt[key])
        self.q[eng].append((waits, fn, key))
        return tok

    def run(self, eng, fns, reads=(), writes=(), dma=None, step=16):
        if not isinstance(fns, list):
            fns = [fns]
        deps = []
        for b in reads:
            deps += b.rd_deps()
        for b in writes:
            deps += b.wr_deps()
        for f in fns[:-1]:
            self.op(eng, f, deps, mark=False)
            deps = ()
        tok = self.op(eng, fns[-1], deps, dma=dma, step=step)
        for b in reads:
            b.did_read(tok)
        for b in writes:
            b.did_write(tok)
        return tok

    def finish(self):
        allt = [(k, v) for k, v in self.cnt.items()]
        for e in self.ENG:
            waits = self._waits(e, allt)
            if waits:
                self.q[e].append((waits, None, None))

    def emit(self):
        nc = self.nc
        self.finish()
        sems = {}
        for k in self.cnt:
            sems[k] = nc.alloc_semaphore(name=self.tag + k)
        with nc.Block() as block:
            for e in self.ENG:
                if not self.q[e]:
                    continue

                def body(eo, e=e):
                    for waits, fn, key in self.q[e]:
                        for (k, v) in waits:
                            eo.wait_ge(sems[k], v)
                        if fn is None:
                            continue
                        ins = fn(eo)
                        if key is not None:
                            ins.then_inc(sems[key], self.step[key])

                getattr(block, self.BLK[e])(body)
        nc.all_engine_barrier()
        nc.clear_and_free_semaphores(list(sems.values()))
        nc.all_engine_barrier()


class Ring:
    def __init__(self, st, nc, name, n, shape, dtype):
        self.t = [st.enter_context(nc.sbuf_tensor(f"{name}{i}", shape, dtype)) for i in range(n)]
        self.b = [Buf() for _ in range(n)]
        self.n = n
        self.i = 0
        self.name = name

    def next(self):
        i = self.i % self.n
        self.i += 1
        return self.t[i], self.b[i], f"{self.name}{i}"


def psum_banks(st, nc, tag, n=8):
    ts = [st.enter_context(nc.psum_tensor(f"{tag}ps{i}", [128, 512], F32)) for i in range(n)]
    return ts, [Buf() for _ in range(n)]


def stage_gather(nc, ncores, pairs):
    S = Sched(nc, "g")
    for i, (ext, bounce, full) in enumerate(pairs):
        b = Buf()
        S.run("pool", lambda e, ext=ext, bounce=bounce: e.dma_start(out=bounce.ap(), in_=ext), writes=[b], dma="b")
        S.run("pool", lambda e, bounce=bounce, full=full: e.collective_compute(
            "AllGather", ALU.bypass, replica_groups=[list(range(ncores))],
            ins=[bounce.ap().opt()], outs=[full.ap().opt()]), reads=[b], dma="cc", step=1)
    S.emit()


def stage_mod(nc, tag, wm, ncol, cT, gT_ap, bmT_ap, bgate_ap, at_out, bt_out, g2_out):
    has_gate = g2_out is not None
    npass = ncol // D
    with ExitStack() as st:
        S = Sched(nc, tag)
        ps, pb = psum_banks(st, nc, tag)
        ring = Ring(st, nc, tag + "wm", 3, [128, D], BF16)
        csT = st.enter_context(nc.sbuf_tensor(tag + "csT", [128, KC], F32))
        c_in = st.enter_context(nc.sbuf_tensor(tag + "cin", [128, KC], F32))
        gT = st.enter_context(nc.sbuf_tensor(tag + "gT", [128, KC], F32))
        bmT = st.enter_context(nc.sbuf_tensor(tag + "bmT", [128, 64], F32))
        modc = st.enter_context(nc.sbuf_tensor(tag + "modc", [128, 64], F32))
        at = st.enter_context(nc.sbuf_tensor(tag + "at", [128, KC], F32))
        bt = st.enter_context(nc.sbuf_tensor(tag + "bt", [128, KC], F32))
        csrep = st.enter_context(nc.sbuf_tensor(tag + "csrep", [128, KC, 128], BF16))
        zeros = st.enter_context(nc.sbuf_tensor(tag + "zeros", [128, 128], F32))
        rowbuf = st.enter_context(nc.sbuf_tensor(tag + "rowbuf", [1, 2 * D], F32))
        one11 = st.enter_context(nc.sbuf_tensor(tag + "one11", [1, 1], F32))
        b_small = Buf()
        b_cs = Buf()
        b_rep = Buf()
        b_row = Buf()
        S.run("sp", lambda e: e.dma_start(out=c_in[:], in_=cT), writes=[b_small], dma="s")
        S.run("sp", lambda e: e.dma_start(out=gT[:], in_=gT_ap), writes=[b_small], dma="s")
        S.run("sp", lambda e: e.dma_start(out=bmT[:], in_=bmT_ap), writes=[b_small], dma="s")
        S.run("act", lambda e: e.activation(out=csT[:], in_=c_in[:], func=AF.Silu), reads=[b_small], writes=[b_cs])
        S.run("dve", lambda e: e.memset(zeros[:], 0.0), writes=[b_rep])
        S.run("dve", lambda e: e.memset(one11[:], 1.0), writes=[b_rep])
        for k in range(KC):
            S.run("dve", lambda e, k=k: e.tensor_scalar(out=csrep[:, k, :], in0=zeros[:], scalar1=csT[:, k:k + 1],
                                                         scalar2=None, op0=ALU.add), reads=[b_cs, b_rep], writes=[b_rep])
        if has_gate:
            ones1b = st.enter_context(nc.sbuf_tensor(tag + "ones1b", [1, 128], BF16))
            bgb = st.enter_context(nc.sbuf_tensor(tag + "bgb", [1, D], BF16))
            g2 = st.enter_context(nc.sbuf_tensor(tag + "g2", [128, D], F32))
            b_g2 = Buf()
            S.run("pool", lambda e: e.dma_start(out=bgb[:], in_=bgate_ap, max_dma_last_dim=8192), writes=[b_rep], dma="cw")
            S.run("dve", lambda e: e.memset(ones1b[:], 1.0), writes=[b_rep])
        for pss in range(npass):
            gate_pass = pss == 2
            for k in range(KC):
                wt, wb, wn = ring.next()
                S.run("pool", lambda e, wt=wt, k=k, pss=pss: e.dma_start(
                    out=wt[:], in_=wm[k * 128:(k + 1) * 128, pss * D:(pss + 1) * D], max_dma_last_dim=8192), writes=[wb], dma=wn)
                fns = [lambda e, wt=wt, k=k, nb=nb, gate_pass=gate_pass: e.matmul(
                    ps[nb][:, :], csrep[:, k, :], wt[:, nb * 512:(nb + 1) * 512], start=(k == 0),
                    stop=(k == KC - 1 and not gate_pass)) for nb in range(8)]
                S.run("pe", fns, reads=[wb, b_rep], writes=pb)
            if gate_pass:
                fns = [lambda e, nb=nb: e.matmul(ps[nb][:, :], ones1b[0:1, :], bgb[0:1, nb * 512:(nb + 1) * 512], start=False, stop=True)
                       for nb in range(8)]
                S.run("pe", fns, reads=[b_rep], writes=pb)
                for nb in range(8):
                    S.run("dve" if nb % 2 else "act", (lambda e, nb=nb: e.tensor_copy(out=g2[:, nb * 512:(nb + 1) * 512], in_=ps[nb][:, :])) if nb % 2 else
                          (lambda e, nb=nb: e.copy(out=g2[:, nb * 512:(nb + 1) * 512], in_=ps[nb][:, :])), reads=[pb[nb]], writes=[b_g2])
            else:
                for nb in range(8):
                    S.run("dve", lambda e, nb=nb, pss=pss: e.tensor_copy(out=rowbuf[0:1, pss * D + nb * 512:pss * D + (nb + 1) * 512], in_=ps[nb][0:1, :]),
                          reads=[pb[nb]], writes=[b_row])
        fns = [lambda e, c=c: e.matmul(ps[0][:, c:c + 1], rowbuf[0:1, c * 128:(c + 1) * 128], one11[0:1, 0:1], start=True, stop=True)
               for c in range(64)]
        S.run("pe", fns, reads=[b_row, b_rep], writes=[pb[0]])
        b_o = Buf()
        S.run("dve", lambda e: e.tensor_tensor(out=modc[:], in0=ps[0][:, 0:64], in1=bmT[:], op=ALU.add),
              reads=[pb[0], b_small], writes=[b_o])
        S.run("dve", lambda e: e.tensor_copy(out=bt[:], in_=modc[:, 0:32]), reads=[b_o], writes=[b_o])
        S.run("dve", lambda e: e.scalar_tensor_tensor(out=at[:], in0=modc[:, 32:64], scalar=1.0, in1=gT[:],
                                                      op0=ALU.add, op1=ALU.mult), reads=[b_o], writes=[b_o])
        S.run("sp", lambda e: e.dma_start(out=at_out, in_=at[:]), reads=[b_o], dma="o")
        S.run("sp", lambda e: e.dma_start(out=bt_out, in_=bt[:]), reads=[b_o], dma="o")
        if has_gate:
            S.run("sp", lambda e: e.dma_start(out=g2_out, in_=g2[:]), reads=[b_g2], dma="o")
        S.emit()


class Common:
    pass


def emit_prologue(S, C, xsrc_tile_ap, m, router=None):
    xt, xb = C.xt, C.xt_b
    S.run("sp", lambda e: e.dma_start(out=xt[:], in_=xsrc_tile_ap), writes=[xb], dma="xt")
    S.run("act", lambda e: e.activation(out=C.junk_ap, in_=xt[:], func=AF.Square, accum_out=C.ss[:]),
          reads=[xb], writes=[C.junk_b, C.ss_b])
    S.run("act", lambda e: e.activation(out=C.ss[:], in_=C.ss[:], func=AF.Sqrt, bias=C.epst[:], scale=1.0 / D),
          reads=[C.ss_b, C.const_b], writes=[C.ss_b])
    S.run("dve", lambda e: e.reciprocal(out=C.rstd[:], in_=C.ss[:]), reads=[C.ss_b], writes=[C.rstd_b])
    S.run("act", lambda e: e.activation(out=xt[:], in_=xt[:], func=AF.Copy, scale=C.rstd[:]),
          reads=[xb, C.rstd_b], writes=[xb])
    for q in range(8):
        bank = C.tp_banks[q % len(C.tp_banks)]
        fns = [lambda e, q=q, i=i, bank=bank: e.transpose(C.ps[bank][:, i * 128:(i + 1) * 128],
                                                          xt[:, (q * 4 + i) * 128:(q * 4 + i + 1) * 128], C.ident[:])
               for i in range(4)]
        S.run("pe", fns, reads=[xb, C.const_b], writes=[C.pb[bank]])
        for i in range(4):
            k = q * 4 + i
            S.run("act", lambda e, k=k, i=i, bank=bank: e.activation(
                out=C.hT[:, k, m * 128:(m + 1) * 128], in_=C.ps[bank][:, i * 128:(i + 1) * 128],
                func=AF.Identity, scale=C.at[:, k:k + 1], bias=C.bt[:, k:k + 1]),
                reads=[C.pb[bank], C.mod_b], writes=[C.hT_b])


def load_common(st, nc, S, tag, at_d, bt_d, g2_d, ident_d, need_g2=True, own_junk=True, npsum=8):
    C = Common()
    C.ps, C.pb = psum_banks(st, nc, tag, npsum)
    C.xt = st.enter_context(nc.sbuf_tensor(tag + "xt", [128, D], F32))
    C.xt_b = Buf()
    if own_junk:
        C.junk = st.enter_context(nc.sbuf_tensor(tag + "junk", [128, D], BF16))
        C.junk_ap = C.junk[:]
        C.junk_b = Buf()
    C.ss = st.enter_context(nc.sbuf_tensor(tag + "ss", [128, 1], F32))
    C.ss_b = Buf()
    C.rstd = st.enter_context(nc.sbuf_tensor(tag + "rstd", [128, 1], F32))
    C.rstd_b = Buf()
    C.epst = st.enter_context(nc.sbuf_tensor(tag + "eps", [128, 1], F32))
    C.ident = st.enter_context(nc.sbuf_tensor(tag + "ident", [128, 128], F32))
    C.at = st.enter_context(nc.sbuf_tensor(tag + "at", [128, KC], F32))
    C.bt = st.enter_context(nc.sbuf_tensor(tag + "bt", [128, KC], F32))
    C.const_b = Buf()
    C.mod_b = Buf()
    S.run("dve", lambda e: e.memset(C.epst[:], EPS), writes=[C.const_b])
    S.run("sp", lambda e: e.dma_start(out=C.ident[:], in_=ident_d), writes=[C.const_b], dma="c")
    S.run("sp", lambda e: e.dma_start(out=C.at[:], in_=at_d), writes=[C.mod_b], dma="c")
    S.run("sp", lambda e: e.dma_start(out=C.bt[:], in_=bt_d), writes=[C.mod_b], dma="c")
    if need_g2:
        C.g2 = st.enter_context(nc.sbuf_tensor(tag + "g2", [128, D], F32))
        C.g2_b = Buf()
        S.run("sp", lambda e: e.dma_start(out=C.g2[:], in_=g2_d), writes=[C.g2_b], dma="c")
    C.hT = st.enter_context(nc.sbuf_tensor(tag + "hT", [128, KC, 512], BF16))
    C.hT_b = Buf()
    C.tp_banks = [6, 7]
    return C


def groups_of(ntiles, g=4):
    out = []
    s = 0
    while s < ntiles:
        out.append((s, min(g, ntiles - s)))
        s += g
    return out


def stage_conv(nc, tag, ntiles, x_src, x_dst, w_in, w_out, wconvT_d, flag_d, at_d, bt_d, g2_d, ident_d):
    with ExitStack() as st:
        S = Sched(nc, tag)
        C = load_common(st, nc, S, tag, at_d, bt_d, g2_d, ident_d)
        ps, pb = C.ps, C.pb
        gT = st.enter_context(nc.sbuf_tensor(tag + "gT", [128, KC, 512], BF16))
        gT_b = Buf()
        wring = Ring(st, nc, tag + "w", 2, [128, KC, 384], BF16)
        wc = st.enter_context(nc.sbuf_tensor(tag + "wc", [128, 3, KC], F32))
        flag = st.enter_context(nc.sbuf_tensor(tag + "flag", [128, 1], F32))
        carry = st.enter_context(nc.sbuf_tensor(tag + "carry", [128, KC, 2], F32))
        carry_b = Buf()
        vring = Ring(st, nc, tag + "v", 2, [128, 514], F32)
        cring = Ring(st, nc, tag + "c", 2, [128, 512], F32)
        aring = Ring(st, nc, tag + "a", 2, [128, 512], F32)
        xring = Ring(st, nc, tag + "xb", 3, [128, 512], F32)
        oring = Ring(st, nc, tag + "ob", 3, [128, 512], F32)
        S.run("sp", lambda e: e.dma_start(out=wc[:], in_=wconvT_d), writes=[C.const_b], dma="c")
        S.run("sp", lambda e: e.dma_start(out=flag[:], in_=flag_d), writes=[C.const_b], dma="c")
        S.run("dve", lambda e: e.memset(carry[:], 0.0), writes=[carry_b])
        for (t0, gt) in groups_of(ntiles):
            T = gt * 128
            for m in range(gt):
                emit_prologue(S, C, x_src[(t0 + m) * 128:(t0 + m + 1) * 128, :], m)
            for i in range(KC):
                banks = (0, 1, 2) if i % 2 == 0 else (3, 4, 5)
                wt, wb, wn = wring.next()
                S.run("pool", lambda e, wt=wt, i=i: e.dma_start(out=wt[:], in_=w_in[i], max_dma_last_dim=8192), writes=[wb], dma=wn)
                for part in range(3):
                    bank = banks[part]
                    fns = [lambda e, wt=wt, k=k, bank=bank, T=T, part=part: e.matmul(ps[bank][:, 0:T], wt[:, k, part * 128:(part + 1) * 128], C.hT[:, k, 0:T],
                                                                            start=(k == 0), stop=(k == KC - 1)) for k in range(KC)]
                    S.run("pe", fns, reads=[wb, C.hT_b], writes=[pb[bank]])
                vt, vb, _ = vring.next()
                ct, cb, _ = cring.next()
                at_, ab, _ = aring.next()
                S.run("act", lambda e, ct=ct, T=T, bk=banks[1]: e.copy(out=ct[:, 0:T], in_=ps[bk][:, 0:T]),
                      reads=[pb[banks[1]]], writes=[cb])
                S.run("dve", lambda e, vt=vt, i=i: e.tensor_copy(out=vt[:, 0:2], in_=carry[:, i, :]),
                      reads=[carry_b], writes=[vb])
                S.run("dve", lambda e, vt=vt, ct=ct, T=T, bk=banks[2]: e.tensor_tensor(
                    out=vt[:, 2:2 + T], in0=ct[:, 0:T], in1=ps[bk][:, 0:T], op=ALU.mult),
                    reads=[cb, pb[banks[2]]], writes=[vb])
                if t0 == 0:
                    S.run("dve", lambda e, vt=vt: e.tensor_scalar(out=vt[:, 2 + 254:2 + 256], in0=vt[:, 2 + 254:2 + 256],
                                                                   scalar1=flag[:, 0:1], scalar2=None, op0=ALU.mult),
                          reads=[vb, C.const_b], writes=[vb])
                S.run("dve", lambda e, vt=vt, i=i, T=T: e.tensor_copy(out=carry[:, i, :], in_=vt[:, T:T + 2]),
                      reads=[vb], writes=[carry_b])
                S.run("dve", lambda e, vt=vt, at_=at_, i=i, T=T: e.tensor_scalar(
                    out=at_[:, 0:T], in0=vt[:, 0:T], scalar1=wc[:, 0, i:i + 1], scalar2=None, op0=ALU.mult),
                    reads=[vb, C.const_b], writes=[ab])
                S.run("dve", lambda e, vt=vt, at_=at_, i=i, T=T: e.scalar_tensor_tensor(
                    out=at_[:, 0:T], in0=vt[:, 1:1 + T], scalar=wc[:, 1, i:i + 1], in1=at_[:, 0:T], op0=ALU.mult, op1=ALU.add),
                    reads=[vb, ab], writes=[ab])
                S.run("dve", lambda e, vt=vt, at_=at_, i=i, T=T: e.scalar_tensor_tensor(
                    out=at_[:, 0:T], in0=vt[:, 2:2 + T], scalar=wc[:, 2, i:i + 1], in1=at_[:, 0:T], op0=ALU.mult, op1=ALU.add),
                    reads=[vb, ab], writes=[ab])
                S.run("dve", lambda e, at_=at_, i=i, T=T, bk=banks[0]: e.tensor_tensor(
                    out=gT[:, i, 0:T], in0=at_[:, 0:T], in1=ps[bk][:, 0:T], op=ALU.mult),
                    reads=[ab, pb[banks[0]]], writes=[gT_b])
            for nb in range(16):
                wt, wb, wn = wring.next()
                S.run("pool", lambda e, wt=wt, nb=nb: e.dma_start(out=wt[:, :, 0:256], in_=w_out[nb], max_dma_last_dim=8192), writes=[wb], dma=wn)
                for m in range(gt):
                    bank = (nb * gt + m) % 6
                    fns = [lambda e, wt=wt, k=k, m=m, bank=bank: e.matmul(ps[bank][:, 0:256], gT[:, k, m * 128:(m + 1) * 128], wt[:, k, 0:256],
                                                                            start=(k == 0), stop=(k == KC - 1)) for k in range(KC)]
                    S.run("pe", fns, reads=[wb, gT_b], writes=[pb[bank]])
                    xb_t, xb_b, xn = xring.next()
                    ob_t, ob_b, on = oring.next()
                    r0 = (t0 + m) * 128
                    S.run("sp", lambda e, xb_t=xb_t, r0=r0, nb=nb: e.dma_start(out=xb_t[:, 0:256], in_=x_src[r0:r0 + 128, nb * 256:(nb + 1) * 256]),
                          writes=[xb_b], dma=xn)
                    S.run("dve", lambda e, ob_t=ob_t, bank=bank, nb=nb: e.tensor_tensor(
                        out=ob_t[:, 0:256], in0=ps[bank][:, 0:256], in1=C.g2[:, nb * 256:(nb + 1) * 256], op=ALU.mult),
                        reads=[pb[bank], C.g2_b], writes=[ob_b])
                    S.run("dve", lambda e, ob_t=ob_t, xb_t=xb_t: e.tensor_tensor(
                        out=ob_t[:, 0:256], in0=ob_t[:, 0:256], in1=xb_t[:, 0:256], op=ALU.add),
                        reads=[xb_b, ob_b], writes=[ob_b])
                    S.run("act", lambda e, ob_t=ob_t, r0=r0, nb=nb: e.dma_start(out=x_dst[r0:r0 + 128, nb * 256:(nb + 1) * 256], in_=ob_t[:, 0:256]),
                          reads=[ob_b], dma=on)
        S.emit()


def stage_moe(nc, tag, tiles, E, x_src, x_dst, w_gu, w_dn, wr_d, br_d, bguT_d, bdn_d, at_d, bt_d, g2_d, ident_d, dst_off=0):
    with ExitStack() as st:
        S = Sched(nc, tag)
        C = load_common(st, nc, S, tag, at_d, bt_d, g2_d, ident_d, own_junk=False)
        ps, pb = C.ps, C.pb
        yacc = st.enter_context(nc.sbuf_tensor(tag + "yacc", [128, 4, D], F32))
        yacc_b = [Buf() for _ in range(4)]
        C.junk_ap = yacc[:, 3, :]
        C.junk_b = yacc_b[3]
        aring = Ring(st, nc, tag + "A", 2, [128, KC, 256], BF16)
        bring = Ring(st, nc, tag + "B", 4, [128, 4, 512], BF16)
        bgu = st.enter_context(nc.sbuf_tensor(tag + "bgu", [128, E, 8], F32))
        bdnring = Ring(st, nc, tag + "bdn", 2, [E, 512], F32)
        G = st.enter_context(nc.sbuf_tensor(tag + "G", [128, 4, E], F32))
        GT = st.enter_context(nc.sbuf_tensor(tag + "GT", [E, 4, 128], F32))
        G_b = Buf()
        lg = st.enter_context(nc.sbuf_tensor(tag + "lg", [128, E], F32))
        ex = st.enter_context(nc.sbuf_tensor(tag + "ex", [128, E], F32))
        mk = st.enter_context(nc.sbuf_tensor(tag + "mk", [128, E], F32))
        m8 = st.enter_context(nc.sbuf_tensor(tag + "m8", [128, 8], F32))
        sm = st.enter_context(nc.sbuf_tensor(tag + "sm", [128, 2], F32))
        r_b = Buf()
        actT = [st.enter_context(nc.sbuf_tensor(tag + f"act{j}", [128, 512], BF16)) for j in range(4)]
        act_b = [Buf() for _ in range(4)]
        t1r = Ring(st, nc, tag + "t1", 2, [128, 512], F32)
        t2r = Ring(st, nc, tag + "t2", 2, [128, 512], F32)
        sgr = Ring(st, nc, tag + "sg", 2, [128, 512], F32)
        S.run("sp", lambda e: e.dma_start(out=bgu[:], in_=bguT_d), writes=[C.const_b], dma="c")
        wrb = st.enter_context(nc.sbuf_tensor(tag + "wrb", [128, KC, E], BF16))
        brb = st.enter_context(nc.sbuf_tensor(tag + "brb", [1, E], BF16))
        ones1b = st.enter_context(nc.sbuf_tensor(tag + "ones1b", [1, 128], BF16))
        S.run("dve", lambda e: e.memset(ones1b[:], 1.0), writes=[C.const_b])
        S.run("pool", lambda e: e.dma_start(out=wrb[:], in_=wr_d), writes=[C.const_b], dma="cw")
        S.run("pool", lambda e: e.dma_start(out=brb[:], in_=br_d), writes=[C.const_b], dma="cw")
        RB = 5
        for (g0, gt) in groups_of(len(tiles)):
            T = gt * 128
            for m in range(gt):
                tix = tiles[g0 + m]
                emit_prologue(S, C, x_src[tix * 128:(tix + 1) * 128, :], m)
                fns = [lambda e, k=k, m=m: e.matmul(ps[RB][:, 0:E], C.hT[:, k, m * 128:(m + 1) * 128], wrb[:, k, :],
                                                    start=(k == 0), stop=False) for k in range(KC)]
                fns.append(lambda e: e.matmul(ps[RB][:, 0:E], ones1b[0:1, :], brb[0:1, :], start=False, stop=True))
                S.run("pe", fns, reads=[C.const_b, C.hT_b], writes=[pb[RB]])
                S.run("dve", lambda e: e.tensor_copy(out=lg[:], in_=ps[RB][:, 0:E]), reads=[pb[RB]], writes=[r_b])
                S.run("dve", lambda e: e.max(out=m8[:], in_=lg[:]), reads=[r_b], writes=[r_b])
                S.run("dve", lambda e: e.tensor_scalar(out=mk[:], in0=lg[:], scalar1=m8[:, 3:4], scalar2=None, op0=ALU.is_ge),
                      reads=[r_b], writes=[r_b])
                S.run("dve", lambda e: e.tensor_scalar(out=sm[:, 0:1], in0=m8[:, 0:1], scalar1=-1.0, scalar2=None, op0=ALU.mult),
                      reads=[r_b], writes=[r_b])
                S.run("act", lambda e: e.activation(out=ex[:], in_=lg[:], func=AF.Exp, bias=sm[:, 0:1], scale=1.0),
                      reads=[r_b], writes=[r_b])
                S.run("dve", lambda e: e.tensor_tensor(out=ex[:], in0=ex[:], in1=mk[:], op=ALU.mult), reads=[r_b], writes=[r_b])
                S.run("dve", lambda e: e.reduce_sum(out=sm[:, 1:2], in_=ex[:], axis=mybir.AxisListType.X), reads=[r_b], writes=[r_b])
                S.run("dve", lambda e: e.reciprocal(out=sm[:, 1:2], in_=sm[:, 1:2]), reads=[r_b], writes=[r_b])
                S.run("dve", lambda e, m=m: e.tensor_scalar(out=G[:, m, :], in0=ex[:], scalar1=sm[:, 1:2], scalar2=None, op0=ALU.mult),
                      reads=[r_b], writes=[G_b])
                S.run("pe", lambda e, m=m: e.transpose(ps[RB][0:E, 128:256], G[:, m, :], C.ident[:]),
                      reads=[G_b, C.const_b], writes=[pb[RB]])
                S.run("dve", lambda e, m=m: e.tensor_copy(out=GT[:, m, :], in_=ps[RB][0:E, 128:256]), reads=[pb[RB]], writes=[G_b])
            STOP = 9
            for n in range(8):
                bt_, bb_, bn_ = bdnring.next()
                S.run("sp", lambda e, bt_=bt_, n=n: e.dma_start(out=bt_[:], in_=bdn_d[:, n * 512:(n + 1) * 512]), writes=[bb_], dma=bn_)
                for m in range(gt):
                    bank = (n * gt + m) % 2 + 6
                    S.run("pe", lambda e, bt_=bt_, m=m, bank=bank: e.matmul(ps[bank][:, :], GT[:, m, :], bt_[:], start=True, stop=True),
                          reads=[G_b, bb_], writes=[pb[bank]])
                    S.run("act", lambda e, m=m, n=n, bank=bank: e.copy(out=yacc[:, m, n * 512:(n + 1) * 512], in_=ps[bank][:, :]),
                          reads=[pb[bank]], writes=[yacc_b[m]])
            for ex_ in range(E if STOP > 2 else 0):
                for j in range(4):
                    wt, wb, wn = aring.next()
                    r0 = ex_ * D
                    S.run("pool", lambda e, wt=wt, ex_=ex_, j=j: e.dma_start(
                        out=wt[:], in_=w_gu(ex_, j), max_dma_last_dim=8192),
                        writes=[wb], dma=wn)
                    bg_, bl_ = (0, 1) if j % 2 == 0 else (2, 3)
                    for half, bank in ((0, bg_), (1, bl_)):
                        fns = [lambda e, wt=wt, k=k, half=half, bank=bank, T=T: e.matmul(
                            ps[bank][:, 0:T], wt[:, k, half * 128:(half + 1) * 128], C.hT[:, k, 0:T],
                            start=(k == 0), stop=(k == KC - 1)) for k in range(KC)]
                        S.run("pe", fns, reads=[wb, C.hT_b], writes=[pb[bank]])
                    t1, t1b, _ = t1r.next()
                    t2, t2b, _ = t2r.next()
                    sg, sgb, _ = sgr.next()
                    S.run("dve", lambda e, t1=t1, T=T, ex_=ex_, j=j, bank=bg_: e.tensor_scalar(
                        out=t1[:, 0:T], in0=ps[bank][:, 0:T], scalar1=bgu[:, ex_, j:j + 1], scalar2=7.0, op0=ALU.add, op1=ALU.min),
                        reads=[pb[bg_], C.const_b], writes=[t1b])
                    S.run("act", lambda e, sg=sg, t1=t1, T=T: e.activation(out=sg[:, 0:T], in_=t1[:, 0:T], func=AF.Sigmoid, scale=1.702),
                          reads=[t1b], writes=[sgb])
                    S.run("dve", lambda e, t2=t2, T=T, ex_=ex_, j=j, bank=bl_: e.tensor_scalar(
                        out=t2[:, 0:T], in0=ps[bank][:, 0:T], scalar1=bgu[:, ex_, 4 + j:5 + j], scalar2=7.0, op0=ALU.add, op1=ALU.min),
                        reads=[pb[bl_], C.const_b], writes=[t2b])
                    S.run("dve", lambda e, t2=t2, T=T: e.tensor_scalar(
                        out=t2[:, 0:T], in0=t2[:, 0:T], scalar1=-7.0, scalar2=1.0, op0=ALU.max, op1=ALU.add),
                        reads=[t2b], writes=[t2b])
                    S.run("dve", lambda e, t1=t1, sg=sg, T=T: e.tensor_tensor(out=t1[:, 0:T], in0=t1[:, 0:T], in1=sg[:, 0:T], op=ALU.mult),
                          reads=[t1b, sgb], writes=[t1b])
                    S.run("dve", lambda e, t1=t1, t2=t2, T=T, j=j: e.tensor_tensor(out=actT[j][:, 0:T], in0=t1[:, 0:T], in1=t2[:, 0:T], op=ALU.mult),
                          reads=[t1b, t2b], writes=[act_b[j]])
                for n in range(8):
                    wt, wb, wn = bring.next()
                    r0 = ex_ * FH
                    S.run("pool", lambda e, wt=wt, ex_=ex_, n=n: e.dma_start(
                        out=wt[:], in_=w_dn(ex_, n), max_dma_last_dim=8192),
                        writes=[wb], dma=wn)
                    for m in range(gt):
                        bank = (n * gt + m) % 2 + 6
                        fns = [lambda e, wt=wt, j=j, m=m, bank=bank: e.matmul(ps[bank][:, :], actT[j][:, m * 128:(m + 1) * 128], wt[:, j, :],
                                                                                start=(j == 0), stop=(j == 3)) for j in range(4)]
                        S.run("pe", fns, reads=[wb] + act_b, writes=[pb[bank]])
                        S.run("dve", lambda e, m=m, n=n, bank=bank, ex_=ex_: e.scalar_tensor_tensor(
                            out=yacc[:, m, n * 512:(n + 1) * 512], in0=ps[bank][:, :], scalar=G[:, m, ex_:ex_ + 1],
                            in1=yacc[:, m, n * 512:(n + 1) * 512], op0=ALU.mult, op1=ALU.add),
                            reads=[pb[bank], G_b, yacc_b[m]], writes=[yacc_b[m]])
            for m in range(gt):
                tix = tiles[g0 + m]
                S.run("sp", lambda e, tix=tix: e.dma_start(out=C.xt[:], in_=x_src[tix * 128:(tix + 1) * 128, :]), writes=[C.xt_b], dma="xt")
                S.run("dve", lambda e, m=m: e.tensor_tensor(out=yacc[:, m, :], in0=yacc[:, m, :], in1=C.g2[:], op=ALU.mult),
                      reads=[C.g2_b, yacc_b[m]], writes=[yacc_b[m]])
                S.run("dve", lambda e, m=m: e.tensor_tensor(out=yacc[:, m, :], in0=yacc[:, m, :], in1=C.xt[:], op=ALU.add),
                      reads=[C.xt_b, yacc_b[m]], writes=[yacc_b[m]])
                S.run("act", lambda e, m=m, tix=tix: e.dma_start(out=x_dst[(tix - dst_off) * 128:(tix - dst_off + 1) * 128, :], in_=yacc[:, m, :]),
                      reads=[yacc_b[m]], writes=[yacc_b[m]], dma="yo")
        S.emit()


def colT(v, nchunk):
    v = np.asarray(v)
    lead = v.shape[:-1]
    return np.ascontiguousarray(np.moveaxis(v.reshape(*lead, nchunk, 128), -1, 0))


def unit256(w):
    n = w.shape[1] // 256
    return np.ascontiguousarray(w.reshape(KC, 128, n, 256).transpose(2, 1, 0, 3))


def prep_weights(inp, E, L=2):
    W = {}
    mods = [inp["w_mod_mix"][0], inp["w_mod_ffn"][0], inp["w_mod_kv"], inp["w_mod_mix"][1], inp["w_mod_ffn"][1]]
    bmods = [inp["b_mod_mix"][0], inp["b_mod_ffn"][0], inp["b_mod_kv"], inp["b_mod_mix"][1], inp["b_mod_ffn"][1]]
    gs = [inp["g_mix"][0], inp["g_ffn"][0], inp["g_kv"], inp["g_mix"][1], inp["g_ffn"][1]]
    for i in range(5):
        W[f"wmod{i}"] = np.ascontiguousarray(mods[i])
        W[f"bmT{i}"] = colT(bmods[i][:2 * D], 64)
        W[f"gT{i}"] = colT(gs[i], KC)
        if mods[i].shape[1] == 3 * D:
            W[f"bg{i}"] = np.ascontiguousarray(bmods[i][2 * D:].reshape(1, D))
    W["w_in"] = np.ascontiguousarray(inp["w_a_in"][0].reshape(KC, 128, 3, KC, 128).transpose(3, 1, 0, 2, 4).reshape(KC, 128, KC, 384))
    W["w_out"] = unit256(inp["w_a_out"][0])
    W["wconvT"] = colT(inp["w_a_conv"][0], KC)
    for l in range(L):
        wgu = inp["w_gu"][l, :E]
        wgu = wgu.reshape(E, KC, 128, 4, 128, 2).transpose(0, 3, 2, 1, 5, 4)
        W[f"w_gu{l}"] = np.ascontiguousarray(wgu.reshape(E, 4, 128, KC, 256))
        wdn = inp["w_dn"][l, :E].reshape(E, 4, 128, 8, 512).transpose(0, 3, 2, 1, 4)
        W[f"w_dn{l}"] = np.ascontiguousarray(wdn)
        W[f"wr{l}"] = np.ascontiguousarray(inp["w_router"][l][:, :E].reshape(KC, 128, E).transpose(1, 0, 2))
        W[f"br{l}"] = np.ascontiguousarray(inp["b_router"][l][:E].reshape(1, E))
        bgu = inp["b_gu"][l][:E]
        glu = bgu[:, 0::2].reshape(E, 4, 128)
        lin = bgu[:, 1::2].reshape(E, 4, 128)
        W[f"bguT{l}"] = np.ascontiguousarray(np.concatenate([glu, lin], axis=1).transpose(2, 0, 1))
        W[f"bdn{l}"] = np.ascontiguousarray(inp["b_dn"][l][:E])
    W["ident"] = np.eye(128, dtype=np.float32)
    return W


SHARDED = ["wmod0", "wmod1", "wmod2", "wmod3", "wmod4", "w_in", "w_out", "w_gu0", "w_dn0", "w_gu1", "w_dn1",
           "w_kv", "w_q", "w_o"]


def qk_norm(S, C, nc_t, pbank, pb_, src_ps, width, bias_col, gcol, scale, out_ap, tmp):
    kf, sq, rr, b = tmp
    S.run("act", lambda e: e.activation(out=kf[:, 0:width], in_=src_ps, func=AF.Identity, bias=bias_col, scale=1.0),
          reads=[pb_[0], C.const_b], writes=[b])
    S.run("act", lambda e: e.activation(out=sq[:, 0:width], in_=kf[:, 0:width], func=AF.Square), reads=[b], writes=[b])
    S.run("pe", lambda e: e.matmul(C.ps[pbank][:, 0:width], C.bdiag[:], sq[:, 0:width], start=True, stop=True),
          reads=[b, C.const_b], writes=[C.pb[pbank]])
    S.run("act", lambda e: e.activation(out=rr[:, 0:width], in_=C.ps[pbank][:, 0:width], func=AF.Sqrt, bias=C.epst[:], scale=1.0 / HD),
          reads=[C.pb[pbank], C.const_b], writes=[b])
    S.run("dve", lambda e: e.reciprocal(out=rr[:, 0:width], in_=rr[:, 0:width]), reads=[b], writes=[b])
    S.run("dve", lambda e: e.tensor_tensor(out=kf[:, 0:width], in0=kf[:, 0:width], in1=rr[:, 0:width], op=ALU.mult), reads=[b], writes=[b])
    return S.run("dve", lambda e: e.tensor_scalar(out=out_ap, in0=kf[:, 0:width], scalar1=gcol, scalar2=float(scale), op0=ALU.mult, op1=ALU.mult),
                 reads=[b, C.const_b], writes=[b])


def stage_kv(nc, tag, tiles, x_src, w_k, w_ksw, w_v, bkT_d, bkswT_d, bv_d, gkT_d, bdiag_d, at_d, bt_d, ident_d, kT_d, kTsw_d, va_d):
    with ExitStack() as st:
        S = Sched(nc, tag)
        C = load_common(st, nc, S, tag, at_d, bt_d, None, ident_d, need_g2=False)
        ps, pb = C.ps, C.pb
        C.bdiag = st.enter_context(nc.sbuf_tensor(tag + "bdiag", [128, 128], F32))
        wk = st.enter_context(nc.sbuf_tensor(tag + "wk", [128, KC, 512], BF16))
        wksw = st.enter_context(nc.sbuf_tensor(tag + "wksw", [128, KC, 512], BF16))
        wv = st.enter_context(nc.sbuf_tensor(tag + "wv", [128, KC, 512], BF16))
        bk = st.enter_context(nc.sbuf_tensor(tag + "bk", [128, 4], F32))
        bksw = st.enter_context(nc.sbuf_tensor(tag + "bksw", [128, 4], F32))
        gk = st.enter_context(nc.sbuf_tensor(tag + "gk", [128, 1], F32))
        bvf = st.enter_context(nc.sbuf_tensor(tag + "bvf", [1, 512], F32))
        bvb = st.enter_context(nc.sbuf_tensor(tag + "bvb", [1, 512], BF16))
        ones1b = st.enter_context(nc.sbuf_tensor(tag + "ones1b", [1, 128], BF16))
        kf = st.enter_context(nc.sbuf_tensor(tag + "kf", [128, 128], F32))
        sq = st.enter_context(nc.sbuf_tensor(tag + "sq", [128, 128], F32))
        rr = st.enter_context(nc.sbuf_tensor(tag + "rr", [128, 128], F32))
        tb = Buf()
        koring = Ring(st, nc, tag + "ko", 2, [128, 128], BF16)
        varing = Ring(st, nc, tag + "va", 2, [128, 8, 65], BF16)
        for (dst, src) in ((wk, w_k), (wksw, w_ksw), (wv, w_v)):
            S.run("pool", lambda e, dst=dst, src=src: e.dma_start(out=dst[:], in_=src.rearrange("(k p) c -> p k c", p=128)),
                  writes=[C.const_b], dma="w")
        for (dst, src) in ((bk, bkT_d), (bksw, bkswT_d), (gk, gkT_d), (bvf, bv_d), (C.bdiag, bdiag_d)):
            S.run("sp", lambda e, dst=dst, src=src: e.dma_start(out=dst[:], in_=src), writes=[C.const_b], dma="c")
        S.run("dve", lambda e: e.tensor_copy(out=bvb[:], in_=bvf[:]), reads=[C.const_b], writes=[C.const_b])
        S.run("dve", lambda e: e.memset(ones1b[:], 1.0), writes=[C.const_b])
        for tix in tiles:
            emit_prologue(S, C, x_src[tix * 128:(tix + 1) * 128, :], 0)
            for (wsel, bsel, dstd) in ((wk, bk, kT_d), (wksw, bksw, kTsw_d)):
                for cc in range(4):
                    bank = cc % 2
                    fns = [lambda e, wsel=wsel, k=k, cc=cc, bank=bank: e.matmul(ps[bank][:, 0:128], wsel[:, k, cc * 128:(cc + 1) * 128], C.hT[:, k, 0:128],
                                                                                  start=(k == 0), stop=(k == KC - 1)) for k in range(KC)]
                    S.run("pe", fns, reads=[C.const_b, C.hT_b], writes=[pb[bank]])
                    ko, kob, kon = koring.next()
                    S.run("dve", lambda e: e.tensor_copy(out=rr[:, 0:1], in_=rr[:, 0:1]), writes=[kob, tb])
                    qk_norm(S, C, nc, 2 + bank, [pb[bank]], ps[bank][:, 0:128], 128, bsel[:, cc:cc + 1], gk[:, 0:1], 1.0, ko[:], (kf, sq, rr, tb))
                    S.run("act", lambda e, ko=ko, dstd=dstd, tix=tix, cc=cc: e.dma_start(out=dstd[tix, cc], in_=ko[:]), reads=[tb], writes=[kob], dma=kon)
            fns = [lambda e, k=k: e.matmul(ps[4][:, :], C.hT[:, k, 0:128], wv[:, k, :], start=(k == 0), stop=False) for k in range(KC)]
            fns.append(lambda e: e.matmul(ps[4][:, :], ones1b[0:1, :], bvb[0:1, :], start=False, stop=True))
            S.run("pe", fns, reads=[C.const_b, C.hT_b], writes=[pb[4]])
            va, vab, van = varing.next()
            S.run("dve", lambda e, va=va: e.memset(va[:], 1.0), writes=[vab])
            S.run("dve", lambda e, va=va: e.tensor_copy(out=va[:, :, 0:64], in_=ps[4][:, :].rearrange("p (g d) -> p g d", d=64)),
                  reads=[pb[4]], writes=[vab])
            S.run("act", lambda e, va=va, tix=tix: e.dma_start(out=va_d[tix], in_=va[:]), reads=[vab], writes=[vab], dma=van)
        S.emit()


def stage_attn(nc, tag, tiles, x_src, x_dst, w_q, w_o, bqT_d, gqT_d, sinkb_d, bo_d, maskp_d, maskc_d, flag_d, bdiag_d,
               kT_d, kTsw_d, va_d, at_d, bt_d, g2_d, ident_d, identb_d):
    with ExitStack() as st:
        S = Sched(nc, tag)
        C = load_common(st, nc, S, tag, at_d, bt_d, g2_d, ident_d, npsum=6)
        ps, pb = C.ps, C.pb
        C.tp_banks = [4, 5]
        C.psb = [st.enter_context(nc.psum_tensor(f"{tag}psb{i}", [128, 1024], BF16)) for i in range(2)]
        pbb = [Buf(), Buf()]
        C.bdiag = st.enter_context(nc.sbuf_tensor(tag + "bdiag", [128, 128], F32))
        identb = st.enter_context(nc.sbuf_tensor(tag + "identb", [128, 128], BF16))
        bq = st.enter_context(nc.sbuf_tensor(tag + "bq", [128, KC], F32))
        gq = st.enter_context(nc.sbuf_tensor(tag + "gq", [128, 1], F32))
        sinke = st.enter_context(nc.sbuf_tensor(tag + "sinke", [128, NH], F32))
        bof = st.enter_context(nc.sbuf_tensor(tag + "bof", [1, D], F32))
        bob = st.enter_context(nc.sbuf_tensor(tag + "bob", [1, D], BF16))
        ones1b = st.enter_context(nc.sbuf_tensor(tag + "ones1b", [1, 128], BF16))
        maskp = st.enter_context(nc.sbuf_tensor(tag + "maskp", [128, 128], BF16))
        maskp0 = st.enter_context(nc.sbuf_tensor(tag + "maskp0", [128, 128], BF16))
        maskc = st.enter_context(nc.sbuf_tensor(tag + "maskc", [128, 128], BF16))
        mf = st.enter_context(nc.sbuf_tensor(tag + "mf", [128, 2, 128], F32))
        flag = st.enter_context(nc.sbuf_tensor(tag + "flag", [128, 1], F32))
        kst = st.enter_context(nc.sbuf_tensor(tag + "kst", [128, 5, 4, 128], BF16))
        ksw = st.enter_context(nc.sbuf_tensor(tag + "ksw", [128, 5, 4, 128], BF16))
        vst = st.enter_context(nc.sbuf_tensor(tag + "vst", [128, 5, 8, 65], BF16))
        kv_b = Buf()
        otok = st.enter_context(nc.sbuf_tensor(tag + "otok", [128, 4, D], BF16))
        otok_b = [Buf() for _ in range(4)]
        qn = st.enter_context(nc.sbuf_tensor(tag + "qn", [128, 512], BF16))
        qn_b = Buf()
        kf = st.enter_context(nc.sbuf_tensor(tag + "kf", [128, 512], F32))
        sq = st.enter_context(nc.sbuf_tensor(tag + "sq", [128, 512], F32))
        rr = st.enter_context(nc.sbuf_tensor(tag + "rr", [128, 512], F32))
        tb = Buf()
        wring = Ring(st, nc, tag + "w", 2, [128, KC, 256], BF16)
        pring = Ring(st, nc, tag + "p", 4, [128, 128], BF16)
        dring = Ring(st, nc, tag + "d", 2, [128, 1], F32)
        xring = Ring(st, nc, tag + "xb", 3, [128, 256], F32)
        oring = Ring(st, nc, tag + "ob", 3, [128, 256], F32)
        for (dst, src) in ((bq[:], bqT_d), (gq[:], gqT_d), (sinke[:], sinkb_d), (bof[:], bo_d), (mf[:, 0, :], maskp_d), (mf[:, 1, :], maskc_d),
                           (flag[:], flag_d), (C.bdiag[:], bdiag_d)):
            S.run("sp", lambda e, dst=dst, src=src: e.dma_start(out=dst, in_=src), writes=[C.const_b], dma="c")
        S.run("pool", lambda e: e.dma_start(out=identb[:], in_=identb_d), writes=[C.const_b], dma="w")
        S.run("act", lambda e: e.activation(out=sinke[:], in_=sinke[:], func=AF.Exp), reads=[C.const_b], writes=[C.const_b])
        S.run("dve", lambda e: e.tensor_copy(out=bob[:], in_=bof[:]), reads=[C.const_b], writes=[C.const_b])
        S.run("dve", lambda e: e.memset(ones1b[:], 1.0), writes=[C.const_b])
        S.run("dve", lambda e: e.tensor_copy(out=maskp[:], in_=mf[:, 0, :]), reads=[C.const_b], writes=[C.const_b])
        S.run("dve", lambda e: e.tensor_copy(out=maskc[:], in_=mf[:, 1, :]), reads=[C.const_b], writes=[C.const_b])
        S.run("dve", lambda e: e.tensor_scalar(out=maskp0[:], in0=mf[:, 0, :], scalar1=flag[:, 0:1], scalar2=None, op0=ALU.mult),
              reads=[C.const_b], writes=[C.const_b])
        first_tile = tiles[0]
        for (g0, gt) in groups_of(len(tiles)):
            T = gt * 128
            tl = tiles[g0:g0 + gt]
            for s in range(gt + 1):
                tix = tl[0] - 1 + s
                S.run("sp", lambda e, s=s, tix=tix: e.dma_start(out=kst[:, s], in_=kT_d[tix].rearrange("c p t -> p c t")), writes=[kv_b], dma="kv")
                S.run("sp", lambda e, s=s, tix=tix: e.dma_start(out=ksw[:, s], in_=kTsw_d[tix].rearrange("c p t -> p c t")), writes=[kv_b], dma="kv")
                S.run("sp", lambda e, s=s, tix=tix: e.dma_start(out=vst[:, s], in_=va_d[tix]), writes=[kv_b], dma="kv")
            for m in range(gt):
                emit_prologue(S, C, x_src[tl[m] * 128:(tl[m] + 1) * 128, :], m)
            for c in range(KC):
                if c % 2 == 0:
                    wt, wb, wn = wring.next()
                    S.run("pool", lambda e, wt=wt, c=c: e.dma_start(
                        out=wt[:], in_=w_q[c // 2], max_dma_last_dim=8192), writes=[wb], dma=wn)
                qb = c % 2
                fns = [lambda e, wt=wt, k=k, T=T, qb=qb, c=c: e.matmul(ps[qb][:, 0:T], wt[:, k, (c % 2) * 128:(c % 2 + 1) * 128], C.hT[:, k, 0:T],
                                                                        start=(k == 0), stop=(k == KC - 1)) for k in range(KC)]
                S.run("pe", fns, reads=[wb, C.hT_b], writes=[pb[qb]])
                S.run("dve", lambda e: e.tensor_copy(out=rr[:, 0:1], in_=rr[:, 0:1]), writes=[qn_b, tb])
                tq = qk_norm(S, C, nc, 2 + qb, [pb[qb]], ps[qb][:, 0:T], T, bq[:, c:c + 1], gq[:, 0:1], HD ** -0.5, qn[:, 0:T], (kf, sq, rr, tb))
                qn_b.did_write(tq)
                g = c // 4
                cc = g // 2
                for m in range(gt):
                    for h in range(2):
                        head = 2 * c + h
                        ksel = kst if (g % 2) == h else ksw
                        obank = 4 + (h % 2)
                        pts = []
                        for kb in range(2):
                            slot = m + kb
                            sbank = 2 + kb
                            S.run("pe", lambda e, ksel=ksel, slot=slot, h=h, m=m, sbank=sbank, cc=cc: e.matmul(
                                ps[sbank][:, 0:128], ksel[h * 64:(h + 1) * 64, slot, cc, :], qn[h * 64:(h + 1) * 64, m * 128:(m + 1) * 128],
                                start=True, stop=True), reads=[kv_b, qn_b], writes=[pb[sbank]])
                            pt, ptb, _ = pring.next()
                            S.run("act", lambda e, pt=pt, sbank=sbank: e.activation(out=pt[:], in_=ps[sbank][:, 0:128], func=AF.Exp),
                                  reads=[pb[sbank]], writes=[ptb])
                            mk_ = maskc if kb == 1 else (maskp0 if tl[m] == first_tile else maskp)
                            S.run("dve", lambda e, pt=pt, mk_=mk_: e.tensor_tensor(out=pt[:], in0=pt[:], in1=mk_[:], op=ALU.mult),
                                  reads=[ptb, C.const_b], writes=[ptb])
                            pts.append((pt, ptb, slot))
                        fns = [lambda e, pt=pt, slot=slot, i=i, obank=obank, g=g: e.matmul(ps[obank][:, 0:65], pt[:], vst[:, slot, g, :],
                                                                                           start=(i == 0), stop=(i == 1))
                               for i, (pt, ptb, slot) in enumerate(pts)]
                        S.run("pe", fns, reads=[pts[0][1], pts[1][1], kv_b], writes=[pb[obank]])
                        dn, dnb, _ = dring.next()
                        S.run("dve", lambda e, dn=dn, obank=obank, head=head: e.tensor_tensor(out=dn[:], in0=ps[obank][:, 64:65], in1=sinke[:, head:head + 1], op=ALU.add),
                              reads=[pb[obank], C.const_b], writes=[dnb])
                        S.run("dve", lambda e, dn=dn: e.reciprocal(out=dn[:], in_=dn[:]), reads=[dnb], writes=[dnb])
                        S.run("dve", lambda e, dn=dn, obank=obank, head=head, m=m: e.tensor_scalar(
                            out=otok[:, m, head * 64:(head + 1) * 64], in0=ps[obank][:, 0:64], scalar1=dn[:, 0:1], scalar2=None, op0=ALU.mult),
                            reads=[pb[obank], dnb], writes=[otok_b[m]])
            for m in range(gt):
                for q in range(8):
                    bank = q % 2
                    fns = [lambda e, q=q, i=i, bank=bank, m=m: e.transpose(C.psb[bank][:, i * 128:(i + 1) * 128],
                                                                            otok[:, m, (q * 4 + i) * 128:(q * 4 + i + 1) * 128], identb[:])
                           for i in range(4)]
                    S.run("pe", fns, reads=[otok_b[m], C.const_b], writes=[pbb[bank]])
                    S.run("act", lambda e, q=q, bank=bank, m=m: e.copy(out=C.hT[:, q * 4:(q + 1) * 4, m * 128:(m + 1) * 128],
                                                                          in_=C.psb[bank][:, 0:512].rearrange("p (i t) -> p i t", t=128)),
                          reads=[pbb[bank]], writes=[C.hT_b])
            for nb in range(16):
                wt, wb, wn = wring.next()
                S.run("pool", lambda e, wt=wt, nb=nb: e.dma_start(
                    out=wt[:], in_=w_o[nb], max_dma_last_dim=8192), writes=[wb], dma=wn)
                for m in range(gt):
                    bank = 2 + (nb * gt + m) % 4
                    fns = [lambda e, wt=wt, k=k, m=m, bank=bank: e.matmul(ps[bank][:, 0:256], C.hT[:, k, m * 128:(m + 1) * 128], wt[:, k, :],
                                                                            start=(k == 0), stop=False) for k in range(KC)]
                    fns.append(lambda e, bank=bank, nb=nb: e.matmul(ps[bank][:, 0:256], ones1b[0:1, :], bob[0:1, nb * 256:(nb + 1) * 256], start=False, stop=True))
                    S.run("pe", fns, reads=[wb, C.hT_b, C.const_b], writes=[pb[bank]])
                    xb_t, xb_b, xn = xring.next()
                    ob_t, ob_b, on = oring.next()
                    r0 = tl[m] * 128
                    S.run("sp", lambda e, xb_t=xb_t, r0=r0, nb=nb: e.dma_start(out=xb_t[:], in_=x_src[r0:r0 + 128, nb * 256:(nb + 1) * 256]), writes=[xb_b], dma=xn)
                    S.run("dve", lambda e, ob_t=ob_t, bank=bank, nb=nb: e.tensor_tensor(out=ob_t[:], in0=ps[bank][:, 0:256], in1=C.g2[:, nb * 256:(nb + 1) * 256], op=ALU.mult),
                          reads=[pb[bank], C.g2_b], writes=[ob_b])
                    S.run("dve", lambda e, ob_t=ob_t, xb_t=xb_t: e.tensor_tensor(out=ob_t[:], in0=ob_t[:], in1=xb_t[:], op=ALU.add), reads=[xb_b, ob_b], writes=[ob_b])
                    S.run("act", lambda e, ob_t=ob_t, r0=r0, nb=nb: e.dma_start(out=x_dst[r0:r0 + 128, nb * 256:(nb + 1) * 256], in_=ob_t[:]), reads=[ob_b], writes=[ob_b], dma=on)
        S.emit()


def prep_attn(inp):
    W = {}
    wkv = inp["w_kv"]
    wk = wkv[:, :512]
    sw = np.array([1, 0, 3, 2, 5, 4, 7, 6])
    W["w_k"] = np.ascontiguousarray(wk)
    W["w_ksw"] = np.ascontiguousarray(wk.reshape(D, 8, 64)[:, sw].reshape(D, 512))
    W["w_v"] = np.ascontiguousarray(wkv[:, 512:])
    bk = inp["b_kv"][:512]
    W["bkT"] = colT(bk, 4)
    W["bkswT"] = colT(bk.reshape(8, 64)[sw].reshape(512), 4)
    W["bv"] = np.ascontiguousarray(inp["b_kv"][512:].reshape(1, 512))
    W["gkT"] = np.ascontiguousarray(np.tile(inp["g_k"], 2).reshape(128, 1))
    W["gqT"] = np.ascontiguousarray(np.tile(inp["g_q"][0], 2).reshape(128, 1))
    bd = np.zeros((128, 128), np.float32)
    bd[:64, :64] = 1.0
    bd[64:, 64:] = 1.0
    W["bdiag"] = bd
    W["w_q"] = unit256(inp["w_b_q"][0])
    W["w_o"] = unit256(inp["w_b_o"][0])
    W["bqT"] = colT(inp["b_b_q"][0], KC)
    W["sinkb"] = np.ascontiguousarray(np.broadcast_to(inp["sinks"][0], (128, NH)))
    W["bo"] = np.ascontiguousarray(inp["b_b_o"][0].reshape(1, D))
    j = np.arange(128)[:, None]
    i = np.arange(128)[None, :]
    W["maskp"] = (j > i).astype(np.float32)
    W["maskc"] = (j <= i).astype(np.float32)
    W["identb"] = np.eye(128, dtype=np.float32)
    return W


NCORES = 8
NT_ALL = 18
E_FULL = 32
PIECES = {"wmod0": 1, "wmod1": 1, "wmod2": 1, "wmod3": 1, "wmod4": 1, "w_in": 1, "w_out": 1, "w_gu0": 4, "w_dn0": 2,
          "w_gu1": 4, "w_dn1": 2, "w_q": 1, "w_o": 1, "w_k": 1, "w_ksw": 1, "w_v": 1}
SMALL = ["bmT0", "bmT1", "bmT2", "bmT3", "bmT4", "gT0", "gT1", "gT2", "gT3", "gT4", "bg0", "bg1", "bg3", "bg4", "wconvT",
         "wr0", "wr1", "br0", "br1", "bguT0", "bguT1", "bdn0", "bdn1", "ident", "identb", "bkT", "bkswT", "bv", "gkT", "gqT",
         "bdiag", "bqT", "sinkb", "bo", "maskp", "maskc"]


def build_full(shapes):
    nc = bass.Bass("TRN2", target_bir_lowering=False)
    ext = {}
    for name, shp in shapes.items():
        ext[name] = nc.dram_tensor(name, list(shp), F32, kind="ExternalInput").ap()
    out = nc.dram_tensor("out", [(NT_ALL - 2) * 128, D], F32, kind="ExternalOutput").ap()
    full = {}
    for name, P in PIECES.items():
        if P == 1:
            full[name] = ext[name]
        else:
            full[name] = [ext[f"{name}_p{p}"] for p in range(P)]
    x1 = nc.dram_tensor("x1", [NT_ALL * 128, D], F32).ap()
    x2 = nc.dram_tensor("x2", [NT_ALL * 128, D], F32).ap()
    x3 = nc.dram_tensor("x3", [NT_ALL * 128, D], F32).ap()
    mods = []
    for i in range(5):
        at_d = nc.dram_tensor(f"at{i}", [128, KC], F32).ap()
        bt_d = nc.dram_tensor(f"bt{i}", [128, KC], F32).ap()
        has_gate = i != 2
        g2_d = nc.dram_tensor(f"g2{i}", [128, D], F32).ap() if has_gate else None
        stage_mod(nc, f"m{i}", full[f"wmod{i}"], 3 * D if has_gate else 2 * D, ext["cT"], ext[f"gT{i}"], ext[f"bmT{i}"],
                  ext[f"bg{i}"] if has_gate else None, at_d, bt_d, g2_d)
        mods.append((at_d, bt_d, g2_d))
    kT_d = nc.dram_tensor("kT_d", [NT_ALL, 4, 128, 128], BF16).ap()
    kTsw_d = nc.dram_tensor("kTsw_d", [NT_ALL, 4, 128, 128], BF16).ap()
    va_d = nc.dram_tensor("va_d", [NT_ALL, 128, 8, 65], BF16).ap()

    def gu_fn(l):
        epp = E_FULL // PIECES[f"w_gu{l}"]
        return lambda e, j: full[f"w_gu{l}"][e // epp][e % epp, j]

    def dn_fn(l):
        epp = E_FULL // PIECES[f"w_dn{l}"]
        return lambda e, n: full[f"w_dn{l}"][e // epp][e % epp, n]

    stage_conv(nc, "s1", NT_ALL, ext["x_in"], x1, full["w_in"], full["w_out"], ext["wconvT"], ext["flag"], *mods[0], ext["ident"])
    stage_moe(nc, "s2", list(range(1, NT_ALL)), E_FULL, x1, x2, gu_fn(0), dn_fn(0), ext["wr0"], ext["br0"], ext["bguT0"], ext["bdn0"],
              *mods[1], ext["ident"])
    stage_kv(nc, "s3", list(range(1, NT_ALL)), x2, full["w_k"], full["w_ksw"], full["w_v"], ext["bkT"], ext["bkswT"], ext["bv"], ext["gkT"],
             ext["bdiag"], mods[2][0], mods[2][1], ext["ident"], kT_d, kTsw_d, va_d)
    stage_attn(nc, "s4", list(range(2, NT_ALL)), x2, x3, full["w_q"], full["w_o"], ext["bqT"], ext["gqT"], ext["sinkb"], ext["bo"], ext["maskp"],
               ext["maskc"], ext["flag"], ext["bdiag"], kT_d, kTsw_d, va_d, *mods[3], ext["ident"], ext["identb"])
    stage_moe(nc, "s5", list(range(2, NT_ALL)), E_FULL, x3, out, gu_fn(1), dn_fn(1), ext["wr1"], ext["br1"], ext["bguT1"], ext["bdn1"],
              *mods[4], ext["ident"], dst_off=2)
    return nc


def shard_rows(Wm, r, P):
    rows, cols = Wm.shape
    return np.ascontiguousarray(Wm.reshape(P, NCORES, rows // (P * NCORES), cols)[:, r].reshape(-1, cols))


TEST_CORES = None


def kernel(**inputs):
    inp = {k: np.asarray(v) for k, v in inputs.items()}
    W = prep_weights(inp, E_FULL)
    W.update(prep_attn(inp))
    x = inp["x"]
    B, SEQ, _ = x.shape
    per = SEQ // 4
    shared = {}
    for name, P in PIECES.items():
        if P == 1:
            shared[name] = W[name]
        else:
            rows = W[name].shape[0] // P
            for p in range(P):
                shared[f"{name}_p{p}"] = np.ascontiguousarray(W[name][p * rows:(p + 1) * rows])
    for name in SMALL:
        shared[name] = W[name]
    ncores = NCORES if TEST_CORES is None else TEST_CORES
    in_maps = []
    for r in range(ncores):
        b, q = r // 4, r % 4
        m = dict(shared)
        xin = np.zeros((NT_ALL * 128, D), np.float32)
        if q > 0:
            xin[:256] = x[b, q * per - 256:q * per]
        xin[256:] = x[b, q * per:(q + 1) * per]
        m["x_in"] = xin
        m["cT"] = colT(inp["c"][b], KC)
        m["flag"] = np.full((128, 1), 1.0 if q > 0 else 0.0, np.float32)
        in_maps.append(m)
    shapes = {k: v.shape for k, v in in_maps[0].items()}
    nc = build_full(shapes)
    res = run_bass_kernel_spmd(nc, in_maps, core_ids=list(range(ncores)))
    out = np.zeros((B, SEQ, D), np.float32)
    for r in range(ncores):
        b, q = r // 4, r % 4
        out[b, q * per:(q + 1) * per] = res.results[r]["out"]
    return out
```

```python
from contextlib import ExitStack
import numpy as np
import concourse.bass as bass
import concourse.mybir as mybir
from concourse.bass_utils import run_bass_kernel_spmd

F32 = mybir.dt.float32
BF16 = mybir.dt.bfloat16
AF = mybir.ActivationFunctionType
ALU = mybir.AluOpType

D = 4096
KC = 32
FH = 512
HD = 64
NH = 64
NKV = 8
EPS = 1e-5


class Buf:
    def __init__(self):
        self.w = {}
        self.r = {}

    def rd_deps(self):
        return list(self.w.items())

    def wr_deps(self):
        return list(self.w.items()) + list(self.r.items())

    def did_read(self, t):
        self.r[t[0]] = max(self.r.get(t[0], 0), t[1])

    def did_write(self, t):
        self.w[t[0]] = max(self.w.get(t[0], 0), t[1])


class Sched:
    ENG = ("pe", "act", "dve", "pool", "sp")
    BLK = {"pe": "tensor", "act": "scalar", "dve": "vector", "pool": "gpsimd", "sp": "sync"}

    def __init__(self, nc, tag):
        self.nc = nc
        self.tag = tag
        self.q = {e: [] for e in self.ENG}
        self.cnt = {}
        self.seen = {e: {} for e in self.ENG}
        self.step = {}

    def _waits(self, eng, deps):
        need = {}
        for d in deps:
            if d is None:
                continue
            need[d[0]] = max(need.get(d[0], 0), d[1])
        out = []
        for k, v in need.items():
            if self.seen[eng].get(k, 0) < v:
                self.seen[eng][k] = v
                out.append((k, v))
        return out

    def op(self, eng, fn, deps=(), mark=True, dma=None, step=16):
        waits = self._waits(eng, deps)
        tok = None
        key = None
        if dma is not None:
            key = "D" + dma
            self.step[key] = step
            self.cnt[key] = self.cnt.get(key, 0) + step
            tok = (key, self.cnt[key])
        elif mark:
            key = "E" + eng
            self.step[key] = 1
            self.cnt[key] = self.cnt.get(key, 0) + 1
            tok = (key, self.cnt[key])
        self.q[eng].append((waits, fn, key))
        return tok

    def run(self, eng, fns, reads=(), writes=(), dma=None, step=16):
        if not isinstance(fns, list):
            fns = [fns]
        deps = []
        for b in reads:
            deps += b.rd_deps()
        for b in writes:
            deps += b.wr_deps()
        for f in fns[:-1]:
            self.op(eng, f, deps, mark=False)
            deps = ()
        tok = self.op(eng, fns[-1], deps, dma=dma, step=step)
        for b in reads:
            b.did_read(tok)
        for b in writes:
            b.did_write(tok)
        return tok

    def finish(self):
        allt = [(k, v) for k, v in self.cnt.items()]
        for e in self.ENG:
            waits = self._waits(e, allt)
            if waits:
                self.q[e].append((waits, None, None))

    def emit(self):
        nc = self.nc
        self.finish()
        sems = {}
        for k in self.cnt:
            sems[k] = nc.alloc_semaphore(name=self.tag + k)
        with nc.Block() as block:
            for e in self.ENG:
                if not self.q[e]:
                    continue

                def body(eo, e=e):
                    for waits, fn, key in self.q[e]:
                        for (k, v) in waits:
                            eo.wait_ge(sems[k], v)
                        if fn is None:
                            continue
                        ins = fn(eo)
                        if key is not None:
                            ins.then_inc(sems[key], self.step[key])

                getattr(block, self.BLK[e])(body)
        nc.all_engine_barrier()
        nc.clear_and_free_semaphores(list(sems.values()))
        nc.all_engine_barrier()


class Ring:
    def __init__(self, st, nc, name, n, shape, dtype):
        self.t = [st.enter_context(nc.sbuf_tensor(f"{name}{i}", shape, dtype)) for i in range(n)]
        self.b = [Buf() for _ in range(n)]
        self.n = n
        self.i = 0
        self.name = name

    def next(self):
        i = self.i % self.n
        self.i += 1
        return self.t[i], self.b[i], f"{self.name}{i}"


def psum_banks(st, nc, tag, n=8):
    ts = [st.enter_context(nc.psum_tensor(f"{tag}ps{i}", [128, 512], F32)) for i in range(n)]
    return ts, [Buf() for _ in range(n)]


def stage_gather(nc, ncores, pairs):
    S = Sched(nc, "g")
    for i, (ext, bounce, full) in enumerate(pairs):
        b = Buf()
        S.run("pool", lambda e, ext=ext, bounce=bounce: e.dma_start(out=bounce.ap(), in_=ext), writes=[b], dma="b")
        S.run("pool", lambda e, bounce=bounce, full=full: e.collective_compute(
            "AllGather", ALU.bypass, replica_groups=[list(range(ncores))],
            ins=[bounce.ap().opt()], outs=[full.ap().opt()]), reads=[b], dma="cc", step=1)
    S.emit()


def stage_mod(nc, tag, wm, ncol, cT, gT_ap, bmT_ap, bgate_ap, at_out, bt_out, g2_out):
    has_gate = g2_out is not None
    npass = ncol // D
    with ExitStack() as st:
        S = Sched(nc, tag)
        ps, pb = psum_banks(st, nc, tag)
        ring = Ring(st, nc, tag + "wm", 3, [128, D], BF16)
        csT = st.enter_context(nc.sbuf_tensor(tag + "csT", [128, KC], F32))
        c_in = st.enter_context(nc.sbuf_tensor(tag + "cin", [128, KC], F32))
        gT = st.enter_context(nc.sbuf_tensor(tag + "gT", [128, KC], F32))
        bmT = st.enter_context(nc.sbuf_tensor(tag + "bmT", [128, 64], F32))
        modc = st.enter_context(nc.sbuf_tensor(tag + "modc", [128, 64], F32))
        at = st.enter_context(nc.sbuf_tensor(tag + "at", [128, KC], F32))
        bt = st.enter_context(nc.sbuf_tensor(tag + "bt", [128, KC], F32))
        csrep = st.enter_context(nc.sbuf_tensor(tag + "csrep", [128, KC, 128], BF16))
        zeros = st.enter_context(nc.sbuf_tensor(tag + "zeros", [128, 128], F32))
        rowbuf = st.enter_context(nc.sbuf_tensor(tag + "rowbuf", [1, 2 * D], F32))
        one11 = st.enter_context(nc.sbuf_tensor(tag + "one11", [1, 1], F32))
        b_small = Buf()
        b_cs = Buf()
        b_rep = Buf()
        b_row = Buf()
        S.run("sp", lambda e: e.dma_start(out=c_in[:], in_=cT), writes=[b_small], dma="s")
        S.run("sp", lambda e: e.dma_start(out=gT[:], in_=gT_ap), writes=[b_small], dma="s")
        S.run("sp", lambda e: e.dma_start(out=bmT[:], in_=bmT_ap), writes=[b_small], dma="s")
        S.run("act", lambda e: e.activation(out=csT[:], in_=c_in[:], func=AF.Silu), reads=[b_small], writes=[b_cs])
        S.run("dve", lambda e: e.memset(zeros[:], 0.0), writes=[b_rep])
        S.run("dve", lambda e: e.memset(one11[:], 1.0), writes=[b_rep])
        for k in range(KC):
            S.run("dve", lambda e, k=k: e.tensor_scalar(out=csrep[:, k, :], in0=zeros[:], scalar1=csT[:, k:k + 1],
                                                         scalar2=None, op0=ALU.add), reads=[b_cs, b_rep], writes=[b_rep])
        if has_gate:
            ones1b = st.enter_context(nc.sbuf_tensor(tag + "ones1b", [1, 128], BF16))
            bgb = st.enter_context(nc.sbuf_tensor(tag + "bgb", [1, D], BF16))
            g2 = st.enter_context(nc.sbuf_tensor(tag + "g2", [128, D], F32))
            b_g2 = Buf()
            S.run("pool", lambda e: e.dma_start(out=bgb[:], in_=bgate_ap, max_dma_last_dim=8192), writes=[b_rep], dma="cw")
            S.run("dve", lambda e: e.memset(ones1b[:], 1.0), writes=[b_rep])
        for pss in range(npass):
            gate_pass = pss == 2
            for k in range(KC):
                wt, wb, wn = ring.next()
                S.run("pool", lambda e, wt=wt, k=k, pss=pss: e.dma_start(
                    out=wt[:], in_=wm[k * 128:(k + 1) * 128, pss * D:(pss + 1) * D], max_dma_last_dim=8192), writes=[wb], dma=wn)
                fns = [lambda e, wt=wt, k=k, nb=nb, gate_pass=gate_pass: e.matmul(
                    ps[nb][:, :], csrep[:, k, :], wt[:, nb * 512:(nb + 1) * 512], start=(k == 0),
                    stop=(k == KC - 1 and not gate_pass)) for nb in range(8)]
                S.run("pe", fns, reads=[wb, b_rep], writes=pb)
            if gate_pass:
                fns = [lambda e, nb=nb: e.matmul(ps[nb][:, :], ones1b[0:1, :], bgb[0:1, nb * 512:(nb + 1) * 512], start=False, stop=True)
                       for nb in range(8)]
                S.run("pe", fns, reads=[b_rep], writes=pb)
                for nb in range(8):
                    S.run("dve" if nb % 2 else "act", (lambda e, nb=nb: e.tensor_copy(out=g2[:, nb * 512:(nb + 1) * 512], in_=ps[nb][:, :])) if nb % 2 else
                          (lambda e, nb=nb: e.copy(out=g2[:, nb * 512:(nb + 1) * 512], in_=ps[nb][:, :])), reads=[pb[nb]], writes=[b_g2])
            else:
                for nb in range(8):
                    S.run("dve", lambda e, nb=nb, pss=pss: e.tensor_copy(out=rowbuf[0:1, pss * D + nb * 512:pss * D + (nb + 1) * 512], in_=ps[nb][0:1, :]),
                          reads=[pb[nb]], writes=[b_row])
        fns = [lambda e, c=c: e.matmul(ps[0][:, c:c + 1], rowbuf[0:1, c * 128:(c + 1) * 128], one11[0:1, 0:1], start=True, stop=True)
               for c in range(64)]
        S.run("pe", fns, reads=[b_row, b_rep], writes=[pb[0]])
        b_o = Buf()
        S.run("dve", lambda e: e.tensor_tensor(out=modc[:], in0=ps[0][:, 0:64], in1=bmT[:], op=ALU.add),
              reads=[pb[0], b_small], writes=[b_o])
        S.run("dve", lambda e: e.tensor_copy(out=bt[:], in_=modc[:, 0:32]), reads=[b_o], writes=[b_o])
        S.run("dve", lambda e: e.scalar_tensor_tensor(out=at[:], in0=modc[:, 32:64], scalar=1.0, in1=gT[:],
                                                      op0=ALU.add, op1=ALU.mult), reads=[b_o], writes=[b_o])
        S.run("sp", lambda e: e.dma_start(out=at_out, in_=at[:]), reads=[b_o], dma="o")
        S.run("sp", lambda e: e.dma_start(out=bt_out, in_=bt[:]), reads=[b_o], dma="o")
        if has_gate:
            S.run("sp", lambda e: e.dma_start(out=g2_out, in_=g2[:]), reads=[b_g2], dma="o")
        S.emit()


class Common:
    pass


def emit_prologue(S, C, xsrc_tile_ap, m, router=None):
    xt, xb = C.xt, C.xt_b
    S.run("sp", lambda e: e.dma_start(out=xt[:], in_=xsrc_tile_ap), writes=[xb], dma="xt")
    S.run("act", lambda e: e.activation(out=C.junk_ap, in_=xt[:], func=AF.Square, accum_out=C.ss[:]),
          reads=[xb], writes=[C.junk_b, C.ss_b])
    S.run("act", lambda e: e.activation(out=C.ss[:], in_=C.ss[:], func=AF.Sqrt, bias=C.epst[:], scale=1.0 / D),
          reads=[C.ss_b, C.const_b], writes=[C.ss_b])
    S.run("dve", lambda e: e.reciprocal(out=C.rstd[:], in_=C.ss[:]), reads=[C.ss_b], writes=[C.rstd_b])
    S.run("act", lambda e: e.activation(out=xt[:], in_=xt[:], func=AF.Copy, scale=C.rstd[:]),
          reads=[xb, C.rstd_b], writes=[xb])
    for q in range(8):
        bank = C.tp_banks[q % len(C.tp_banks)]
        fns = [lambda e, q=q, i=i, bank=bank: e.transpose(C.ps[bank][:, i * 128:(i + 1) * 128],
                                                          xt[:, (q * 4 + i) * 128:(q * 4 + i + 1) * 128], C.ident[:])
               for i in range(4)]
        S.run("pe", fns, reads=[xb, C.const_b], writes=[C.pb[bank]])
        for i in range(4):
            k = q * 4 + i
            S.run("act", lambda e, k=k, i=i, bank=bank: e.activation(
                out=C.hT[:, k, m * 128:(m + 1) * 128], in_=C.ps[bank][:, i * 128:(i + 1) * 128],
                func=AF.Identity, scale=C.at[:, k:k + 1], bias=C.bt[:, k:k + 1]),
                reads=[C.pb[bank], C.mod_b], writes=[C.hT_b])


def load_common(st, nc, S, tag, at_d, bt_d, g2_d, ident_d, need_g2=True, own_junk=True, npsum=8):
    C = Common()
    C.ps, C.pb = psum_banks(st, nc, tag, npsum)
    C.xt = st.enter_context(nc.sbuf_tensor(tag + "xt", [128, D], F32))
    C.xt_b = Buf()
    if own_junk:
        C.junk = st.enter_context(nc.sbuf_tensor(tag + "junk", [128, D], BF16))
        C.junk_ap = C.junk[:]
        C.junk_b = Buf()
    C.ss = st.enter_context(nc.sbuf_tensor(tag + "ss", [128, 1], F32))
    C.ss_b = Buf()
    C.rstd = st.enter_context(nc.sbuf_tensor(tag + "rstd", [128, 1], F32))
    C.rstd_b = Buf()
    C.epst = st.enter_context(nc.sbuf_tensor(tag + "eps", [128, 1], F32))
    C.ident = st.enter_context(nc.sbuf_tensor(tag + "ident", [128, 128], F32))
    C.at = st.enter_context(nc.sbuf_tensor(tag + "at", [128, KC], F32))
    C.bt = st.enter_context(nc.sbuf_tensor(tag + "bt", [128, KC], F32))
    C.const_b = Buf()
    C.mod_b = Buf()
    S.run("dve", lambda e: e.memset(C.epst[:], EPS), writes=[C.const_b])
    S.run("sp", lambda e: e.dma_start(out=C.ident[:], in_=ident_d), writes=[C.const_b], dma="c")
    S.run("sp", lambda e: e.dma_start(out=C.at[:], in_=at_d), writes=[C.mod_b], dma="c")
    S.run("sp", lambda e: e.dma_start(out=C.bt[:], in_=bt_d), writes=[C.mod_b], dma="c")
    if need_g2:
        C.g2 = st.enter_context(nc.sbuf_tensor(tag + "g2", [128, D], F32))
        C.g2_b = Buf()
        S.run("sp", lambda e: e.dma_start(out=C.g2[:], in_=g2_d), writes=[C.g2_b], dma="c")
    C.hT = st.enter_context(nc.sbuf_tensor(tag + "hT", [128, KC, 512], BF16))
    C.hT_b = Buf()
    C.tp_banks = [6, 7]
    return C


def groups_of(ntiles, g=4):
    out = []
    s = 0
    while s < ntiles:
        out.append((s, min(g, ntiles - s)))
        s += g
    return out


def stage_conv(nc, tag, ntiles, x_src, x_dst, w_in, w_out, wconvT_d, flag_d, at_d, bt_d, g2_d, ident_d):
    with ExitStack() as st:
        S = Sched(nc, tag)
        C = load_common(st, nc, S, tag, at_d, bt_d, g2_d, ident_d)
        ps, pb = C.ps, C.pb
        gT = st.enter_context(nc.sbuf_tensor(tag + "gT", [128, KC, 512], BF16))
        gT_b = Buf()
        wring = Ring(st, nc, tag + "w", 2, [128, KC, 384], BF16)
        wc = st.enter_context(nc.sbuf_tensor(tag + "wc", [128, 3, KC], F32))
        flag = st.enter_context(nc.sbuf_tensor(tag + "flag", [128, 1], F32))
        carry = st.enter_context(nc.sbuf_tensor(tag + "carry", [128, KC, 2], F32))
        carry_b = Buf()
        vring = Ring(st, nc, tag + "v", 2, [128, 514], F32)
        cring = Ring(st, nc, tag + "c", 2, [128, 512], F32)
        aring = Ring(st, nc, tag + "a", 2, [128, 512], F32)
        xring = Ring(st, nc, tag + "xb", 3, [128, 512], F32)
        oring = Ring(st, nc, tag + "ob", 3, [128, 512], F32)
        S.run("sp", lambda e: e.dma_start(out=wc[:], in_=wconvT_d), writes=[C.const_b], dma="c")
        S.run("sp", lambda e: e.dma_start(out=flag[:], in_=flag_d), writes=[C.const_b], dma="c")
        S.run("dve", lambda e: e.memset(carry[:], 0.0), writes=[carry_b])
        for (t0, gt) in groups_of(ntiles):
            T = gt * 128
            for m in range(gt):
                emit_prologue(S, C, x_src[(t0 + m) * 128:(t0 + m + 1) * 128, :], m)
            for i in range(KC):
                banks = (0, 1, 2) if i % 2 == 0 else (3, 4, 5)
                wt, wb, wn = wring.next()
                S.run("pool", lambda e, wt=wt, i=i: e.dma_start(out=wt[:], in_=w_in[i], max_dma_last_dim=8192), writes=[wb], dma=wn)
                for part in range(3):
                    bank = banks[part]
                    fns = [lambda e, wt=wt, k=k, bank=bank, T=T, part=part: e.matmul(ps[bank][:, 0:T], wt[:, k, part * 128:(part + 1) * 128], C.hT[:, k, 0:T],
                                                                            start=(k == 0), stop=(k == KC - 1)) for k in range(KC)]
                    S.run("pe", fns, reads=[wb, C.hT_b], writes=[pb[bank]])
                vt, vb, _ = vring.next()
                ct, cb, _ = cring.next()
                at_, ab, _ = aring.next()
                S.run("act", lambda e, ct=ct, T=T, bk=banks[1]: e.copy(out=ct[:, 0:T], in_=ps[bk][:, 0:T]),
                      reads=[pb[banks[1]]], writes=[cb])
                S.run("dve", lambda e, vt=vt, i=i: e.tensor_copy(out=vt[:, 0:2], in_=carry[:, i, :]),
                      reads=[carry_b], writes=[vb])
                S.run("dve", lambda e, vt=vt, ct=ct, T=T, bk=banks[2]: e.tensor_tensor(
                    out=vt[:, 2:2 + T], in0=ct[:, 0:T], in1=ps[bk][:, 0:T], op=ALU.mult),
                    reads=[cb, pb[banks[2]]], writes=[vb])
                if t0 == 0:
                    S.run("dve", lambda e, vt=vt: e.tensor_scalar(out=vt[:, 2 + 254:2 + 256], in0=vt[:, 2 + 254:2 + 256],
                                                                   scalar1=flag[:, 0:1], scalar2=None, op0=ALU.mult),
                          reads=[vb, C.const_b], writes=[vb])
                S.run("dve", lambda e, vt=vt, i=i, T=T: e.tensor_copy(out=carry[:, i, :], in_=vt[:, T:T + 2]),
                      reads=[vb], writes=[carry_b])
                S.run("dve", lambda e, vt=vt, at_=at_, i=i, T=T: e.tensor_scalar(
                    out=at_[:, 0:T], in0=vt[:, 0:T], scalar1=wc[:, 0, i:i + 1], scalar2=None, op0=ALU.mult),
                    reads=[vb, C.const_b], writes=[ab])
                S.run("dve", lambda e, vt=vt, at_=at_, i=i, T=T: e.scalar_tensor_tensor(
                    out=at_[:, 0:T], in0=vt[:, 1:1 + T], scalar=wc[:, 1, i:i + 1], in1=at_[:, 0:T], op0=ALU.mult, op1=ALU.add),
                    reads=[vb, ab], writes=[ab])
                S.run("dve", lambda e, vt=vt, at_=at_, i=i, T=T: e.scalar_tensor_tensor(
                    out=at_[:, 0:T], in0=vt[:, 2:2 + T], scalar=wc[:, 2, i:i + 1], in1=at_[:, 0:T], op0=ALU.mult, op1=ALU.add),
                    reads=[vb, ab], writes=[ab])
                S.run("dve", lambda e, at_=at_, i=i, T=T, bk=banks[0]: e.tensor_tensor(
                    out=gT[:, i, 0:T], in0=at_[:, 0:T], in1=ps[bk][:, 0:T], op=ALU.mult),
                    reads=[ab, pb[banks[0]]], writes=[gT_b])
            for nb in range(16):
                wt, wb, wn = wring.next()
                S.run("pool", lambda e, wt=wt, nb=nb: e.dma_start(out=wt[:, :, 0:256], in_=w_out[nb], max_dma_last_dim=8192), writes=[wb], dma=wn)
                for m in range(gt):
                    bank = (nb * gt + m) % 6
                    fns = [lambda e, wt=wt, k=k, m=m, bank=bank: e.matmul(ps[bank][:, 0:256], gT[:, k, m * 128:(m + 1) * 128], wt[:, k, 0:256],
                                                                            start=(k == 0), stop=(k == KC - 1)) for k in range(KC)]
                    S.run("pe", fns, reads=[wb, gT_b], writes=[pb[bank]])
                    xb_t, xb_b, xn = xring.next()
                    ob_t, ob_b, on = oring.next()
                    r0 = (t0 + m) * 128
                    S.run("sp", lambda e, xb_t=xb_t, r0=r0, nb=nb: e.dma_start(out=xb_t[:, 0:256], in_=x_src[r0:r0 + 128, nb * 256:(nb + 1) * 256]),
                          writes=[xb_b], dma=xn)
                    S.run("dve", lambda e, ob_t=ob_t, bank=bank, nb=nb: e.tensor_tensor(
                        out=ob_t[:, 0:256], in0=ps[bank][:, 0:256], in1=C.g2[:, nb * 256:(nb + 1) * 256], op=ALU.mult),
                        reads=[pb[bank], C.g2_b], writes=[ob_b])
                    S.run("dve", lambda e, ob_t=ob_t, xb_t=xb_t: e.tensor_tensor(
                        out=ob_t[:, 0:256], in0=ob_t[:, 0:256], in1=xb_t[:, 0:256], op=ALU.add),
                        reads=[xb_b, ob_b], writes=[ob_b])
                    S.run("act", lambda e, ob_t=ob_t, r0=r0, nb=nb: e.dma_start(out=x_dst[r0:r0 + 128, nb * 256:(nb + 1) * 256], in_=ob_t[:, 0:256]),
                          reads=[ob_b], dma=on)
        S.emit()


def stage_moe(nc, tag, tiles, E, x_src, x_dst, w_gu, w_dn, wr_d, br_d, bguT_d, bdn_d, at_d, bt_d, g2_d, ident_d, dst_off=0):
    with ExitStack() as st:
        S = Sched(nc, tag)
        C = load_common(st, nc, S, tag, at_d, bt_d, g2_d, ident_d, own_junk=False)
        ps, pb = C.ps, C.pb
        yacc = st.enter_context(nc.sbuf_tensor(tag + "yacc", [128, 4, D], F32))
        yacc_b = [Buf() for _ in range(4)]
        C.junk_ap = yacc[:, 3, :]
        C.junk_b = yacc_b[3]
        aring = Ring(st, nc, tag + "A", 2, [128, KC, 256], BF16)
        bring = Ring(st, nc, tag + "B", 4, [128, 4, 512], BF16)
        bgu = st.enter_context(nc.sbuf_tensor(tag + "bgu", [128, E, 8], F32))
        bdnring = Ring(st, nc, tag + "bdn", 2, [E, 512], F32)
        G = st.enter_context(nc.sbuf_tensor(tag + "G", [128, 4, E], F32))
        GT = st.enter_context(nc.sbuf_tensor(tag + "GT", [E, 4, 128], F32))
        G_b = Buf()
        lg = st.enter_context(nc.sbuf_tensor(tag + "lg", [128, E], F32))
        ex = st.enter_context(nc.sbuf_tensor(tag + "ex", [128, E], F32))
        mk = st.enter_context(nc.sbuf_tensor(tag + "mk", [128, E], F32))
        m8 = st.enter_context(nc.sbuf_tensor(tag + "m8", [128, 8], F32))
        sm = st.enter_context(nc.sbuf_tensor(tag + "sm", [128, 2], F32))
        r_b = Buf()
        actT = [st.enter_context(nc.sbuf_tensor(tag + f"act{j}", [128, 512], BF16)) for j in range(4)]
        act_b = [Buf() for _ in range(4)]
        t1r = Ring(st, nc, tag + "t1", 2, [128, 512], F32)
        t2r = Ring(st, nc, tag + "t2", 2, [128, 512], F32)
        sgr = Ring(st, nc, tag + "sg", 2, [128, 512], F32)
        S.run("sp", lambda e: e.dma_start(out=bgu[:], in_=bguT_d), writes=[C.const_b], dma="c")
        wrb = st.enter_context(nc.sbuf_tensor(tag + "wrb", [128, KC, E], BF16))
        brb = st.enter_context(nc.sbuf_tensor(tag + "brb", [1, E], BF16))
        ones1b = st.enter_context(nc.sbuf_tensor(tag + "ones1b", [1, 128], BF16))
        S.run("dve", lambda e: e.memset(ones1b[:], 1.0), writes=[C.const_b])
        S.run("pool", lambda e: e.dma_start(out=wrb[:], in_=wr_d), writes=[C.const_b], dma="cw")
        S.run("pool", lambda e: e.dma_start(out=brb[:], in_=br_d), writes=[C.const_b], dma="cw")
        RB = 5
        for (g0, gt) in groups_of(len(tiles)):
            T = gt * 128
            for m in range(gt):
                tix = tiles[g0 + m]
                emit_prologue(S, C, x_src[tix * 128:(tix + 1) * 128, :], m)
                fns = [lambda e, k=k, m=m: e.matmul(ps[RB][:, 0:E], C.hT[:, k, m * 128:(m + 1) * 128], wrb[:, k, :],
                                                    start=(k == 0), stop=False) for k in range(KC)]
                fns.append(lambda e: e.matmul(ps[RB][:, 0:E], ones1b[0:1, :], brb[0:1, :], start=False, stop=True))
                S.run("pe", fns, reads=[C.const_b, C.hT_b], writes=[pb[RB]])
                S.run("dve", lambda e: e.tensor_copy(out=lg[:], in_=ps[RB][:, 0:E]), reads=[pb[RB]], writes=[r_b])
                S.run("dve", lambda e: e.max(out=m8[:], in_=lg[:]), reads=[r_b], writes=[r_b])
                S.run("dve", lambda e: e.tensor_scalar(out=mk[:], in0=lg[:], scalar1=m8[:, 3:4], scalar2=None, op0=ALU.is_ge),
                      reads=[r_b], writes=[r_b])
                S.run("dve", lambda e: e.tensor_scalar(out=sm[:, 0:1], in0=m8[:, 0:1], scalar1=-1.0, scalar2=None, op0=ALU.mult),
                      reads=[r_b], writes=[r_b])
                S.run("act", lambda e: e.activation(out=ex[:], in_=lg[:], func=AF.Exp, bias=sm[:, 0:1], scale=1.0),
                      reads=[r_b], writes=[r_b])
                S.run("dve", lambda e: e.tensor_tensor(out=ex[:], in0=ex[:], in1=mk[:], op=ALU.mult), reads=[r_b], writes=[r_b])
                S.run("dve", lambda e: e.reduce_sum(out=sm[:, 1:2], in_=ex[:], axis=mybir.AxisListType.X), reads=[r_b], writes=[r_b])
                S.run("dve", lambda e: e.reciprocal(out=sm[:, 1:2], in_=sm[:, 1:2]), reads=[r_b], writes=[r_b])
                S.run("dve", lambda e, m=m: e.tensor_scalar(out=G[:, m, :], in0=ex[:], scalar1=sm[:, 1:2], scalar2=None, op0=ALU.mult),
                      reads=[r_b], writes=[G_b])
                S.run("pe", lambda e, m=m: e.transpose(ps[RB][0:E, 128:256], G[:, m, :], C.ident[:]),
                      reads=[G_b, C.const_b], writes=[pb[RB]])
                S.run("dve", lambda e, m=m: e.tensor_copy(out=GT[:, m, :], in_=ps[RB][0:E, 128:256]), reads=[pb[RB]], writes=[G_b])
            STOP = 9
            for n in range(8):
                bt_, bb_, bn_ = bdnring.next()
                S.run("sp", lambda e, bt_=bt_, n=n: e.dma_start(out=bt_[:], in_=bdn_d[:, n * 512:(n + 1) * 512]), writes=[bb_], dma=bn_)
                for m in range(gt):
                    bank = (n * gt + m) % 2 + 6
                    S.run("pe", lambda e, bt_=bt_, m=m, bank=bank: e.matmul(ps[bank][:, :], GT[:, m, :], bt_[:], start=True, stop=True),
                          reads=[G_b, bb_], writes=[pb[bank]])
                    S.run("act", lambda e, m=m, n=n, bank=bank: e.copy(out=yacc[:, m, n * 512:(n + 1) * 512], in_=ps[bank][:, :]),
                          reads=[pb[bank]], writes=[yacc_b[m]])
            for ex_ in range(E if STOP > 2 else 0):
                for j in range(4):
                    wt, wb, wn = aring.next()
                    r0 = ex_ * D
                    S.run("pool", lambda e, wt=wt, ex_=ex_, j=j: e.dma_start(
                        out=wt[:], in_=w_gu(ex_, j), max_dma_last_dim=8192),
                        writes=[wb], dma=wn)
                    bg_, bl_ = (0, 1) if j % 2 == 0 else (2, 3)
                    for half, bank in ((0, bg_), (1, bl_)):
                        fns = [lambda e, wt=wt, k=k, half=half, bank=bank, T=T: e.matmul(
                            ps[bank][:, 0:T], wt[:, k, half * 128:(half + 1) * 128], C.hT[:, k, 0:T],
                            start=(k == 0), stop=(k == KC - 1)) for k in range(KC)]
                        S.run("pe", fns, reads=[wb, C.hT_b], writes=[pb[bank]])
                    t1, t1b, _ = t1r.next()
                    t2, t2b, _ = t2r.next()
                    sg, sgb, _ = sgr.next()
                    S.run("dve", lambda e, t1=t1, T=T, ex_=ex_, j=j, bank=bg_: e.tensor_scalar(
                        out=t1[:, 0:T], in0=ps[bank][:, 0:T], scalar1=bgu[:, ex_, j:j + 1], scalar2=7.0, op0=ALU.add, op1=ALU.min),
                        reads=[pb[bg_], C.const_b], writes=[t1b])
                    S.run("act", lambda e, sg=sg, t1=t1, T=T: e.activation(out=sg[:, 0:T], in_=t1[:, 0:T], func=AF.Sigmoid, scale=1.702),
                          reads=[t1b], writes=[sgb])
                    S.run("dve", lambda e, t2=t2, T=T, ex_=ex_, j=j, bank=bl_: e.tensor_scalar(
                        out=t2[:, 0:T], in0=ps[bank][:, 0:T], scalar1=bgu[:, ex_, 4 + j:5 + j], scalar2=7.0, op0=ALU.add, op1=ALU.min),
                        reads=[pb[bl_], C.const_b], writes=[t2b])
                    S.run("dve", lambda e, t2=t2, T=T: e.tensor_scalar(
                        out=t2[:, 0:T], in0=t2[:, 0:T], scalar1=-7.0, scalar2=1.0, op0=ALU.max, op1=ALU.add),
                        reads=[t2b], writes=[t2b])
                    S.run("dve", lambda e, t1=t1, sg=sg, T=T: e.tensor_tensor(out=t1[:, 0:T], in0=t1[:, 0:T], in1=sg[:, 0:T], op=ALU.mult),
                          reads=[t1b, sgb], writes=[t1b])
                    S.run("dve", lambda e, t1=t1, t2=t2, T=T, j=j: e.tensor_tensor(out=actT[j][:, 0:T], in0=t1[:, 0:T], in1=t2[:, 0:T], op=ALU.mult),
                          reads=[t1b, t2b], writes=[act_b[j]])
                for n in range(8):
                    wt, wb, wn = bring.next()
                    r0 = ex_ * FH
                    S.run("pool", lambda e, wt=wt, ex_=ex_, n=n: e.dma_start(
                        out=wt[:], in_=w_dn(ex_, n), max_dma_last_dim=8192),
                        writes=[wb], dma=wn)
                    for m in range(gt):
                        bank = (n * gt + m) % 2 + 6
                        fns = [lambda e, wt=wt, j=j, m=m, bank=bank: e.matmul(ps[bank][:, :], actT[j][:, m * 128:(m + 1) * 128], wt[:, j, :],
                                                                                start=(j == 0), stop=(j == 3)) for j in range(4)]
                        S.run("pe", fns, reads=[wb] + act_b, writes=[pb[bank]])
                        S.run("dve", lambda e, m=m, n=n, bank=bank, ex_=ex_: e.scalar_tensor_tensor(
                            out=yacc[:, m, n * 512:(n + 1) * 512], in0=ps[bank][:, :], scalar=G[:, m, ex_:ex_ + 1],
                            in1=yacc[:, m, n * 512:(n + 1) * 512], op0=ALU.mult, op1=ALU.add),
                            reads=[pb[bank], G_b, yacc_b[m]], writes=[yacc_b[m]])
            for m in range(gt):
                tix = tiles[g0 + m]
                S.run("sp", lambda e, tix=tix: e.dma_start(out=C.xt[:], in_=x_src[tix * 128:(tix + 1) * 128, :]), writes=[C.xt_b], dma="xt")
                S.run("dve", lambda e, m=m: e.tensor_tensor(out=yacc[:, m, :], in0=yacc[:, m, :], in1=C.g2[:], op=ALU.mult),
                      reads=[C.g2_b, yacc_b[m]], writes=[yacc_b[m]])
                S.run("dve", lambda e, m=m: e.tensor_tensor(out=yacc[:, m, :], in0=yacc[:, m, :], in1=C.xt[:], op=ALU.add),
                      reads=[C.xt_b, yacc_b[m]], writes=[yacc_b[m]])
                S.run("act", lambda e, m=m, tix=tix: e.dma_start(out=x_dst[(tix - dst_off) * 128:(tix - dst_off + 1) * 128, :], in_=yacc[:, m, :]),
                      reads=[yacc_b[m]], writes=[yacc_b[m]], dma="yo")
        S.emit()


def colT(v, nchunk):
    v = np.asarray(v)
    lead = v.shape[:-1]
    return np.ascontiguousarray(np.moveaxis(v.reshape(*lead, nchunk, 128), -1, 0))


def unit256(w):
    n = w.shape[1] // 256
    return np.ascontiguousarray(w.reshape(KC, 128, n, 256).transpose(2, 1, 0, 3))


def prep_weights(inp, E, L=2):
    W = {}
    mods = [inp["w_mod_mix"][0], inp["w_mod_ffn"][0], inp["w_mod_kv"], inp["w_mod_mix"][1], inp["w_mod_ffn"][1]]
    bmods = [inp["b_mod_mix"][0], inp["b_mod_ffn"][0], inp["b_mod_kv"], inp["b_mod_mix"][1], inp["b_mod_ffn"][1]]
    gs = [inp["g_mix"][0], inp["g_ffn"][0], inp["g_kv"], inp["g_mix"][1], inp["g_ffn"][1]]
    for i in range(5):
        W[f"wmod{i}"] = np.ascontiguousarray(mods[i])
        W[f"bmT{i}"] = colT(bmods[i][:2 * D], 64)
        W[f"gT{i}"] = colT(gs[i], KC)
        if mods[i].shape[1] == 3 * D:
            W[f"bg{i}"] = np.ascontiguousarray(bmods[i][2 * D:].reshape(1, D))
    W["w_in"] = np.ascontiguousarray(inp["w_a_in"][0].reshape(KC, 128, 3, KC, 128).transpose(3, 1, 0, 2, 4).reshape(KC, 128, KC, 384))
    W["w_out"] = unit256(inp["w_a_out"][0])
    W["wconvT"] = colT(inp["w_a_conv"][0], KC)
    for l in range(L):
        wgu = inp["w_gu"][l, :E]
        wgu = wgu.reshape(E, KC, 128, 4, 128, 2).transpose(0, 3, 2, 1, 5, 4)
        W[f"w_gu{l}"] = np.ascontiguousarray(wgu.reshape(E, 4, 128, KC, 256))
        wdn = inp["w_dn"][l, :E].reshape(E, 4, 128, 8, 512).transpose(0, 3, 2, 1, 4)
        W[f"w_dn{l}"] = np.ascontiguousarray(wdn)
        W[f"wr{l}"] = np.ascontiguousarray(inp["w_router"][l][:, :E].reshape(KC, 128, E).transpose(1, 0, 2))
        W[f"br{l}"] = np.ascontiguousarray(inp["b_router"][l][:E].reshape(1, E))
        bgu = inp["b_gu"][l][:E]
        glu = bgu[:, 0::2].reshape(E, 4, 128)
        lin = bgu[:, 1::2].reshape(E, 4, 128)
        W[f"bguT{l}"] = np.ascontiguousarray(np.concatenate([glu, lin], axis=1).transpose(2, 0, 1))
        W[f"bdn{l}"] = np.ascontiguousarray(inp["b_dn"][l][:E])
    W["ident"] = np.eye(128, dtype=np.float32)
    return W


SHARDED = ["wmod0", "wmod1", "wmod2", "wmod3", "wmod4", "w_in", "w_out", "w_gu0", "w_dn0", "w_gu1", "w_dn1",
           "w_kv", "w_q", "w_o"]


def qk_norm(S, C, nc_t, pbank, pb_, src_ps, width, bias_col, gcol, scale, out_ap, tmp):
    kf, sq, rr, b = tmp
    S.run("act", lambda e: e.activation(out=kf[:, 0:width], in_=src_ps, func=AF.Identity, bias=bias_col, scale=1.0),
          reads=[pb_[0], C.const_b], writes=[b])
    S.run("act", lambda e: e.activation(out=sq[:, 0:width], in_=kf[:, 0:width], func=AF.Square), reads=[b], writes=[b])
    S.run("pe", lambda e: e.matmul(C.ps[pbank][:, 0:width], C.bdiag[:], sq[:, 0:width], start=True, stop=True),
          reads=[b, C.const_b], writes=[C.pb[pbank]])
    S.run("act", lambda e: e.activation(out=rr[:, 0:width], in_=C.ps[pbank][:, 0:width], func=AF.Sqrt, bias=C.epst[:], scale=1.0 / HD),
          reads=[C.pb[pbank], C.const_b], writes=[b])
    S.run("dve", lambda e: e.reciprocal(out=rr[:, 0:width], in_=rr[:, 0:width]), reads=[b], writes=[b])
    S.run("dve", lambda e: e.tensor_tensor(out=kf[:, 0:width], in0=kf[:, 0:width], in1=rr[:, 0:width], op=ALU.mult), reads=[b], writes=[b])
    return S.run("dve", lambda e: e.tensor_scalar(out=out_ap, in0=kf[:, 0:width], scalar1=gcol, scalar2=float(scale), op0=ALU.mult, op1=ALU.mult),
                 reads=[b, C.const_b], writes=[b])


def stage_kv(nc, tag, tiles, x_src, w_k, w_ksw, w_v, bkT_d, bkswT_d, bv_d, gkT_d, bdiag_d, at_d, bt_d, ident_d, kT_d, kTsw_d, va_d):
    with ExitStack() as st:
        S = Sched(nc, tag)
        C = load_common(st, nc, S, tag, at_d, bt_d, None, ident_d, need_g2=False)
        ps, pb = C.ps, C.pb
        C.bdiag = st.enter_context(nc.sbuf_tensor(tag + "bdiag", [128, 128], F32))
        wk = st.enter_context(nc.sbuf_tensor(tag + "wk", [128, KC, 512], BF16))
        wksw = st.enter_context(nc.sbuf_tensor(tag + "wksw", [128, KC, 512], BF16))
        wv = st.enter_context(nc.sbuf_tensor(tag + "wv", [128, KC, 512], BF16))
        bk = st.enter_context(nc.sbuf_tensor(tag + "bk", [128, 4], F32))
        bksw = st.enter_context(nc.sbuf_tensor(tag + "bksw", [128, 4], F32))
        gk = st.enter_context(nc.sbuf_tensor(tag + "gk", [128, 1], F32))
        bvf = st.enter_context(nc.sbuf_tensor(tag + "bvf", [1, 512], F32))
        bvb = st.enter_context(nc.sbuf_tensor(tag + "bvb", [1, 512], BF16))
        ones1b = st.enter_context(nc.sbuf_tensor(tag + "ones1b", [1, 128], BF16))
        kf = st.enter_context(nc.sbuf_tensor(tag + "kf", [128, 128], F32))
        sq = st.enter_context(nc.sbuf_tensor(tag + "sq", [128, 128], F32))
        rr = st.enter_context(nc.sbuf_tensor(tag + "rr", [128, 128], F32))
        tb = Buf()
        koring = Ring(st, nc, tag + "ko", 2, [128, 128], BF16)
        varing = Ring(st, nc, tag + "va", 2, [128, 8, 65], BF16)
        for (dst, src) in ((wk, w_k), (wksw, w_ksw), (wv, w_v)):
            S.run("pool", lambda e, dst=dst, src=src: e.dma_start(out=dst[:], in_=src.rearrange("(k p) c -> p k c", p=128)),
                  writes=[C.const_b], dma="w")
        for (dst, src) in ((bk, bkT_d), (bksw, bkswT_d), (gk, gkT_d), (bvf, bv_d), (C.bdiag, bdiag_d)):
            S.run("sp", lambda e, dst=dst, src=src: e.dma_start(out=dst[:], in_=src), writes=[C.const_b], dma="c")
        S.run("dve", lambda e: e.tensor_copy(out=bvb[:], in_=bvf[:]), reads=[C.const_b], writes=[C.const_b])
        S.run("dve", lambda e: e.memset(ones1b[:], 1.0), writes=[C.const_b])
        for tix in tiles:
            emit_prologue(S, C, x_src[tix * 128:(tix + 1) * 128, :], 0)
            for (wsel, bsel, dstd) in ((wk, bk, kT_d), (wksw, bksw, kTsw_d)):
                for cc in range(4):
                    bank = cc % 2
                    fns = [lambda e, wsel=wsel, k=k, cc=cc, bank=bank: e.matmul(ps[bank][:, 0:128], wsel[:, k, cc * 128:(cc + 1) * 128], C.hT[:, k, 0:128],
                                                                                  start=(k == 0), stop=(k == KC - 1)) for k in range(KC)]
                    S.run("pe", fns, reads=[C.const_b, C.hT_b], writes=[pb[bank]])
                    ko, kob, kon = koring.next()
                    S.run("dve", lambda e: e.tensor_copy(out=rr[:, 0:1], in_=rr[:, 0:1]), writes=[kob, tb])
                    qk_norm(S, C, nc, 2 + bank, [pb[bank]], ps[bank][:, 0:128], 128, bsel[:, cc:cc + 1], gk[:, 0:1], 1.0, ko[:], (kf, sq, rr, tb))
                    S.run("act", lambda e, ko=ko, dstd=dstd, tix=tix, cc=cc: e.dma_start(out=dstd[tix, cc], in_=ko[:]), reads=[tb], writes=[kob], dma=kon)
            fns = [lambda e, k=k: e.matmul(ps[4][:, :], C.hT[:, k, 0:128], wv[:, k, :], start=(k == 0), stop=False) for k in range(KC)]
            fns.append(lambda e: e.matmul(ps[4][:, :], ones1b[0:1, :], bvb[0:1, :], start=False, stop=True))
            S.run("pe", fns, reads=[C.const_b, C.hT_b], writes=[pb[4]])
            va, vab, van = varing.next()
            S.run("dve", lambda e, va=va: e.memset(va[:], 1.0), writes=[vab])
            S.run("dve", lambda e, va=va: e.tensor_copy(out=va[:, :, 0:64], in_=ps[4][:, :].rearrange("p (g d) -> p g d", d=64)),
                  reads=[pb[4]], writes=[vab])
            S.run("act", lambda e, va=va, tix=tix: e.dma_start(out=va_d[tix], in_=va[:]), reads=[vab], writes=[vab], dma=van)
        S.emit()


def stage_attn(nc, tag, tiles, x_src, x_dst, w_q, w_o, bqT_d, gqT_d, sinkb_d, bo_d, maskp_d, maskc_d, flag_d, bdiag_d,
               kT_d, kTsw_d, va_d, at_d, bt_d, g2_d, ident_d, identb_d):
    with ExitStack() as st:
        S = Sched(nc, tag)
        C = load_common(st, nc, S, tag, at_d, bt_d, g2_d, ident_d, npsum=6)
        ps, pb = C.ps, C.pb
        C.tp_banks = [4, 5]
        C.psb = [st.enter_context(nc.psum_tensor(f"{tag}psb{i}", [128, 1024], BF16)) for i in range(2)]
        pbb = [Buf(), Buf()]
        C.bdiag = st.enter_context(nc.sbuf_tensor(tag + "bdiag", [128, 128], F32))
        identb = st.enter_context(nc.sbuf_tensor(tag + "identb", [128, 128], BF16))
        bq = st.enter_context(nc.sbuf_tensor(tag + "bq", [128, KC], F32))
        gq = st.enter_context(nc.sbuf_tensor(tag + "gq", [128, 1], F32))
        sinke = st.enter_context(nc.sbuf_tensor(tag + "sinke", [128, NH], F32))
        bof = st.enter_context(nc.sbuf_tensor(tag + "bof", [1, D], F32))
        bob = st.enter_context(nc.sbuf_tensor(tag + "bob", [1, D], BF16))
        ones1b = st.enter_context(nc.sbuf_tensor(tag + "ones1b", [1, 128], BF16))
        maskp = st.enter_context(nc.sbuf_tensor(tag + "maskp", [128, 128], BF16))
        maskp0 = st.enter_context(nc.sbuf_tensor(tag + "maskp0", [128, 128], BF16))
        maskc = st.enter_context(nc.sbuf_tensor(tag + "maskc", [128, 128], BF16))
        mf = st.enter_context(nc.sbuf_tensor(tag + "mf", [128, 2, 128], F32))
        flag = st.enter_context(nc.sbuf_tensor(tag + "flag", [128, 1], F32))
        kst = st.enter_context(nc.sbuf_tensor(tag + "kst", [128, 5, 4, 128], BF16))
        ksw = st.enter_context(nc.sbuf_tensor(tag + "ksw", [128, 5, 4, 128], BF16))
        vst = st.enter_context(nc.sbuf_tensor(tag + "vst", [128, 5, 8, 65], BF16))
        kv_b = Buf()
        otok = st.enter_context(nc.sbuf_tensor(tag + "otok", [128, 4, D], BF16))
        otok_b = [Buf() for _ in range(4)]
        qn = st.enter_context(nc.sbuf_tensor(tag + "qn", [128, 512], BF16))
        qn_b = Buf()
        kf = st.enter_context(nc.sbuf_tensor(tag + "kf", [128, 512], F32))
        sq = st.enter_context(nc.sbuf_tensor(tag + "sq", [128, 512], F32))
        rr = st.enter_context(nc.sbuf_tensor(tag + "rr", [128, 512], F32))
        tb = Buf()
        wring = Ring(st, nc, tag + "w", 2, [128, KC, 256], BF16)
        pring = Ring(st, nc, tag + "p", 8, [128, 128], BF16)
        dring = Ring(st, nc, tag + "d", 4, [128, 1], F32)
        xring = Ring(st, nc, tag + "xb", 3, [128, 256], F32)
        oring = Ring(st, nc, tag + "ob", 3, [128, 256], F32)
        for (dst, src) in ((bq[:], bqT_d), (gq[:], gqT_d), (sinke[:], sinkb_d), (bof[:], bo_d), (mf[:, 0, :], maskp_d), (mf[:, 1, :], maskc_d),
                           (flag[:], flag_d), (C.bdiag[:], bdiag_d)):
            S.run("sp", lambda e, dst=dst, src=src: e.dma_start(out=dst, in_=src), writes=[C.const_b], dma="c")
        S.run("pool", lambda e: e.dma_start(out=identb[:], in_=identb_d), writes=[C.const_b], dma="w")
        S.run("act", lambda e: e.activation(out=sinke[:], in_=sinke[:], func=AF.Exp), reads=[C.const_b], writes=[C.const_b])
        S.run("dve", lambda e: e.tensor_copy(out=bob[:], in_=bof[:]), reads=[C.const_b], writes=[C.const_b])
        S.run("dve", lambda e: e.memset(ones1b[:], 1.0), writes=[C.const_b])
        S.run("dve", lambda e: e.tensor_copy(out=maskp[:], in_=mf[:, 0, :]), reads=[C.const_b], writes=[C.const_b])
        S.run("dve", lambda e: e.tensor_copy(out=maskc[:], in_=mf[:, 1, :]), reads=[C.const_b], writes=[C.const_b])
        S.run("dve", lambda e: e.tensor_scalar(out=maskp0[:], in0=mf[:, 0, :], scalar1=flag[:, 0:1], scalar2=None, op0=ALU.mult),
              reads=[C.const_b], writes=[C.const_b])
        first_tile = tiles[0]
        for (g0, gt) in groups_of(len(tiles)):
            T = gt * 128
            tl = tiles[g0:g0 + gt]
            for s in range(gt + 1):
                tix = tl[0] - 1 + s
                S.run("sp", lambda e, s=s, tix=tix: e.dma_start(out=kst[:, s], in_=kT_d[tix].rearrange("c p t -> p c t")), writes=[kv_b], dma="kv")
                S.run("sp", lambda e, s=s, tix=tix: e.dma_start(out=ksw[:, s], in_=kTsw_d[tix].rearrange("c p t -> p c t")), writes=[kv_b], dma="kv")
                S.run("sp", lambda e, s=s, tix=tix: e.dma_start(out=vst[:, s], in_=va_d[tix]), writes=[kv_b], dma="kv")
            for m in range(gt):
                emit_prologue(S, C, x_src[tl[m] * 128:(tl[m] + 1) * 128, :], m)
            for c in range(KC):
                if c % 2 == 0:
                    wt, wb, wn = wring.next()
                    S.run("pool", lambda e, wt=wt, c=c: e.dma_start(
                        out=wt[:], in_=w_q[c // 2], max_dma_last_dim=8192), writes=[wb], dma=wn)
                qb = c % 2
                fns = [lambda e, wt=wt, k=k, T=T, qb=qb, c=c: e.matmul(ps[qb][:, 0:T], wt[:, k, (c % 2) * 128:(c % 2 + 1) * 128], C.hT[:, k, 0:T],
                                                                        start=(k == 0), stop=(k == KC - 1)) for k in range(KC)]
                S.run("pe", fns, reads=[wb, C.hT_b], writes=[pb[qb]])
                S.run("dve", lambda e: e.tensor_copy(out=rr[:, 0:1], in_=rr[:, 0:1]), writes=[qn_b, tb])
                tq = qk_norm(S, C, nc, 2 + qb, [pb[qb]], ps[qb][:, 0:T], T, bq[:, c:c + 1], gq[:, 0:1], HD ** -0.5, qn[:, 0:T], (kf, sq, rr, tb))
                qn_b.did_write(tq)
                g = c // 4
                cc = g // 2
                def emit_scores(it, m, h):
                    ksel = kst if (g % 2) == h else ksw
                    pts = []
                    for kb in range(2):
                        slot = m + kb
                        sbank = (2 if it % 2 == 0 else 0) + kb
                        S.run("pe", lambda e, ksel=ksel, slot=slot, h=h, m=m, sbank=sbank, cc=cc: e.matmul(
                            ps[sbank][:, 0:128], ksel[h * 64:(h + 1) * 64, slot, cc, :], qn[h * 64:(h + 1) * 64, m * 128:(m + 1) * 128],
                            start=True, stop=True), reads=[kv_b, qn_b], writes=[pb[sbank]])
                        pt, ptb, _ = pring.next()
                        S.run("act", lambda e, pt=pt, sbank=sbank: e.activation(out=pt[:], in_=ps[sbank][:, 0:128], func=AF.Exp),
                              reads=[pb[sbank]], writes=[ptb])
                        mk_ = maskc if kb == 1 else (maskp0 if tl[m] == first_tile else maskp)
                        S.run("dve", lambda e, pt=pt, mk_=mk_: e.tensor_tensor(out=pt[:], in0=pt[:], in1=mk_[:], op=ALU.mult),
                              reads=[ptb, C.const_b], writes=[ptb])
                        pts.append((pt, ptb, slot))
                    return pts

                def emit_pv(it, m, h, pts):
                    head = 2 * c + h
                    obank = 4 + (it % 2)
                    fns = [lambda e, pt=pt, slot=slot, i=i, obank=obank, g=g: e.matmul(ps[obank][:, 0:65], pt[:], vst[:, slot, g, :],
                                                                                   start=(i == 0), stop=(i == 1))
                           for i, (pt, ptb, slot) in enumerate(pts)]
                    S.run("pe", fns, reads=[pts[0][1], pts[1][1], kv_b], writes=[pb[obank]])
                    dn, dnb, _ = dring.next()
                    S.run("dve", lambda e, dn=dn, obank=obank, head=head: e.tensor_tensor(out=dn[:], in0=ps[obank][:, 64:65], in1=sinke[:, head:head + 1], op=ALU.add),
                          reads=[pb[obank], C.const_b], writes=[dnb])
                    S.run("dve", lambda e, dn=dn: e.reciprocal(out=dn[:], in_=dn[:]), reads=[dnb], writes=[dnb])
                    S.run("dve", lambda e, dn=dn, obank=obank, head=head, m=m: e.tensor_scalar(
                        out=otok[:, m, head * 64:(head + 1) * 64], in0=ps[obank][:, 0:64], scalar1=dn[:, 0:1], scalar2=None, op0=ALU.mult),
                        reads=[pb[obank], dnb], writes=[otok_b[m]])

                prev = None
                for it, (m, h) in enumerate([(m, h) for m in range(gt) for h in range(2)]):
                    pts = emit_scores(it, m, h)
                    if prev is not None:
                        emit_pv(*prev)
                    prev = (it, m, h, pts)
                emit_pv(*prev)
            for m in range(gt):
                for q in range(8):
                    bank = q % 2
                    fns = [lambda e, q=q, i=i, bank=bank, m=m: e.transpose(C.psb[bank][:, i * 128:(i + 1) * 128],
                                                                            otok[:, m, (q * 4 + i) * 128:(q * 4 + i + 1) * 128], identb[:])
                           for i in range(4)]
                    S.run("pe", fns, reads=[otok_b[m], C.const_b], writes=[pbb[bank]])
                    S.run("act", lambda e, q=q, bank=bank, m=m: e.copy(out=C.hT[:, q * 4:(q + 1) * 4, m * 128:(m + 1) * 128],
                                                                          in_=C.psb[bank][:, 0:512].rearrange("p (i t) -> p i t", t=128)),
                          reads=[pbb[bank]], writes=[C.hT_b])
            for nb in range(16):
                wt, wb, wn = wring.next()
                S.run("pool", lambda e, wt=wt, nb=nb: e.dma_start(
                    out=wt[:], in_=w_o[nb], max_dma_last_dim=8192), writes=[wb], dma=wn)
                for m in range(gt):
                    bank = 2 + (nb * gt + m) % 4
                    fns = [lambda e, wt=wt, k=k, m=m, bank=bank: e.matmul(ps[bank][:, 0:256], C.hT[:, k, m * 128:(m + 1) * 128], wt[:, k, :],
                                                                            start=(k == 0), stop=False) for k in range(KC)]
                    fns.append(lambda e, bank=bank, nb=nb: e.matmul(ps[bank][:, 0:256], ones1b[0:1, :], bob[0:1, nb * 256:(nb + 1) * 256], start=False, stop=True))
                    S.run("pe", fns, reads=[wb, C.hT_b, C.const_b], writes=[pb[bank]])
                    xb_t, xb_b, xn = xring.next()
                    ob_t, ob_b, on = oring.next()
                    r0 = tl[m] * 128
                    S.run("sp", lambda e, xb_t=xb_t, r0=r0, nb=nb: e.dma_start(out=xb_t[:], in_=x_src[r0:r0 + 128, nb * 256:(nb + 1) * 256]), writes=[xb_b], dma=xn)
                    S.run("dve", lambda e, ob_t=ob_t, bank=bank, nb=nb: e.tensor_tensor(out=ob_t[:], in0=ps[bank][:, 0:256], in1=C.g2[:, nb * 256:(nb + 1) * 256], op=ALU.mult),
                          reads=[pb[bank], C.g2_b], writes=[ob_b])
                    S.run("dve", lambda e, ob_t=ob_t, xb_t=xb_t: e.tensor_tensor(out=ob_t[:], in0=ob_t[:], in1=xb_t[:], op=ALU.add), reads=[xb_b, ob_b], writes=[ob_b])
                    S.run("act", lambda e, ob_t=ob_t, r0=r0, nb=nb: e.dma_start(out=x_dst[r0:r0 + 128, nb * 256:(nb + 1) * 256], in_=ob_t[:]), reads=[ob_b], writes=[ob_b], dma=on)
        S.emit()


def prep_attn(inp):
    W = {}
    wkv = inp["w_kv"]
    wk = wkv[:, :512]
    sw = np.array([1, 0, 3, 2, 5, 4, 7, 6])
    W["w_k"] = np.ascontiguousarray(wk)
    W["w_ksw"] = np.ascontiguousarray(wk.reshape(D, 8, 64)[:, sw].reshape(D, 512))
    W["w_v"] = np.ascontiguousarray(wkv[:, 512:])
    bk = inp["b_kv"][:512]
    W["bkT"] = colT(bk, 4)
    W["bkswT"] = colT(bk.reshape(8, 64)[sw].reshape(512), 4)
    W["bv"] = np.ascontiguousarray(inp["b_kv"][512:].reshape(1, 512))
    W["gkT"] = np.ascontiguousarray(np.tile(inp["g_k"], 2).reshape(128, 1))
    W["gqT"] = np.ascontiguousarray(np.tile(inp["g_q"][0], 2).reshape(128, 1))
    bd = np.zeros((128, 128), np.float32)
    bd[:64, :64] = 1.0
    bd[64:, 64:] = 1.0
    W["bdiag"] = bd
    W["w_q"] = unit256(inp["w_b_q"][0])
    W["w_o"] = unit256(inp["w_b_o"][0])
    W["bqT"] = colT(inp["b_b_q"][0], KC)
    W["sinkb"] = np.ascontiguousarray(np.broadcast_to(inp["sinks"][0], (128, NH)))
    W["bo"] = np.ascontiguousarray(inp["b_b_o"][0].reshape(1, D))
    j = np.arange(128)[:, None]
    i = np.arange(128)[None, :]
    W["maskp"] = (j > i).astype(np.float32)
    W["maskc"] = (j <= i).astype(np.float32)
    W["identb"] = np.eye(128, dtype=np.float32)
    return W


NCORES = 8
NT_ALL = 18
E_FULL = 32
PIECES = {"wmod0": 1, "wmod1": 1, "wmod2": 1, "wmod3": 1, "wmod4": 1, "w_in": 1, "w_out": 1, "w_gu0": 4, "w_dn0": 2,
          "w_gu1": 4, "w_dn1": 2, "w_q": 1, "w_o": 1, "w_k": 1, "w_ksw": 1, "w_v": 1}
SMALL = ["bmT0", "bmT1", "bmT2", "bmT3", "bmT4", "gT0", "gT1", "gT2", "gT3", "gT4", "bg0", "bg1", "bg3", "bg4", "wconvT",
         "wr0", "wr1", "br0", "br1", "bguT0", "bguT1", "bdn0", "bdn1", "ident", "identb", "bkT", "bkswT", "bv", "gkT", "gqT",
         "bdiag", "bqT", "sinkb", "bo", "maskp", "maskc"]


def build_full(shapes):
    nc = bass.Bass("TRN2", target_bir_lowering=False)
    ext = {}
    for name, shp in shapes.items():
        ext[name] = nc.dram_tensor(name, list(shp), F32, kind="ExternalInput").ap()
    out = nc.dram_tensor("out", [(NT_ALL - 2) * 128, D], F32, kind="ExternalOutput").ap()
    full = {}
    for name, P in PIECES.items():
        if P == 1:
            full[name] = ext[name]
        else:
            full[name] = [ext[f"{name}_p{p}"] for p in range(P)]
    x1 = nc.dram_tensor("x1", [NT_ALL * 128, D], F32).ap()
    x2 = nc.dram_tensor("x2", [NT_ALL * 128, D], F32).ap()
    x3 = nc.dram_tensor("x3", [NT_ALL * 128, D], F32).ap()
    mods = []
    for i in range(5):
        at_d = nc.dram_tensor(f"at{i}", [128, KC], F32).ap()
        bt_d = nc.dram_tensor(f"bt{i}", [128, KC], F32).ap()
        has_gate = i != 2
        g2_d = nc.dram_tensor(f"g2{i}", [128, D], F32).ap() if has_gate else None
        stage_mod(nc, f"m{i}", full[f"wmod{i}"], 3 * D if has_gate else 2 * D, ext["cT"], ext[f"gT{i}"], ext[f"bmT{i}"],
                  ext[f"bg{i}"] if has_gate else None, at_d, bt_d, g2_d)
        mods.append((at_d, bt_d, g2_d))
    kT_d = nc.dram_tensor("kT_d", [NT_ALL, 4, 128, 128], BF16).ap()
    kTsw_d = nc.dram_tensor("kTsw_d", [NT_ALL, 4, 128, 128], BF16).ap()
    va_d = nc.dram_tensor("va_d", [NT_ALL, 128, 8, 65], BF16).ap()

    def gu_fn(l):
        epp = E_FULL // PIECES[f"w_gu{l}"]
        return lambda e, j: full[f"w_gu{l}"][e // epp][e % epp, j]

    def dn_fn(l):
        epp = E_FULL // PIECES[f"w_dn{l}"]
        return lambda e, n: full[f"w_dn{l}"][e // epp][e % epp, n]

    stage_conv(nc, "s1", NT_ALL, ext["x_in"], x1, full["w_in"], full["w_out"], ext["wconvT"], ext["flag"], *mods[0], ext["ident"])
    stage_moe(nc, "s2", list(range(1, NT_ALL)), E_FULL, x1, x2, gu_fn(0), dn_fn(0), ext["wr0"], ext["br0"], ext["bguT0"], ext["bdn0"],
              *mods[1], ext["ident"])
    stage_kv(nc, "s3", list(range(1, NT_ALL)), x2, full["w_k"], full["w_ksw"], full["w_v"], ext["bkT"], ext["bkswT"], ext["bv"], ext["gkT"],
             ext["bdiag"], mods[2][0], mods[2][1], ext["ident"], kT_d, kTsw_d, va_d)
    stage_attn(nc, "s4", list(range(2, NT_ALL)), x2, x3, full["w_q"], full["w_o"], ext["bqT"], ext["gqT"], ext["sinkb"], ext["bo"], ext["maskp"],
               ext["maskc"], ext["flag"], ext["bdiag"], kT_d, kTsw_d, va_d, *mods[3], ext["ident"], ext["identb"])
    stage_moe(nc, "s5", list(range(2, NT_ALL)), E_FULL, x3, out, gu_fn(1), dn_fn(1), ext["wr1"], ext["br1"], ext["bguT1"], ext["bdn1"],
              *mods[4], ext["ident"], dst_off=2)
    return nc


def shard_rows(Wm, r, P):
    rows, cols = Wm.shape
    return np.ascontiguousarray(Wm.reshape(P, NCORES, rows // (P * NCORES), cols)[:, r].reshape(-1, cols))


TEST_CORES = None


def kernel(**inputs):
    inp = {k: np.asarray(v) for k, v in inputs.items()}
    W = prep_weights(inp, E_FULL)
    W.update(prep_attn(inp))
    x = inp["x"]
    B, SEQ, _ = x.shape
    per = SEQ // 4
    shared = {}
    for name, P in PIECES.items():
        if P == 1:
            shared[name] = W[name]
        else:
            rows = W[name].shape[0] // P
            for p in range(P):
                shared[f"{name}_p{p}"] = np.ascontiguousarray(W[name][p * rows:(p + 1) * rows])
    for name in SMALL:
        shared[name] = W[name]
    ncores = NCORES if TEST_CORES is None else TEST_CORES
    in_maps = []
    for r in range(ncores):
        b, q = r // 4, r % 4
        m = dict(shared)
        xin = np.zeros((NT_ALL * 128, D), np.float32)
        if q > 0:
            xin[:256] = x[b, q * per - 256:q * per]
        xin[256:] = x[b, q * per:(q + 1) * per]
        m["x_in"] = xin
        m["cT"] = colT(inp["c"][b], KC)
        m["flag"] = np.full((128, 1), 1.0 if q > 0 else 0.0, np.float32)
        in_maps.append(m)
    shapes = {k: v.shape for k, v in in_maps[0].items()}
    nc = build_full(shapes)
    res = run_bass_kernel_spmd(nc, in_maps, core_ids=list(range(ncores)))
    out = np.zeros((B, SEQ, D), np.float32)
    for r in range(ncores):
        b, q = r // 4, r % 4
        out[b, q * per:(q + 1) * per] = res.results[r]["out"]
    return out
```
